# Optimizing a Trainium2 kernel written in Bass

```python
import jax, jax.numpy as jnp
from jax import lax
import numpy as np

D_MODEL = 1024
BATCH = 32
SEQ = 2048
DEPTH = 1

HG_HEADS = 4
HG_DK = 128
HG_DV = 128
HG_CHUNK = 64
HG_QK_WIDTH = HG_HEADS * HG_DK
HG_V_WIDTH = HG_HEADS * HG_DV
ATT_Q_HEADS = 8
ATT_KV_HEADS = 2
ATT_GROUP = ATT_Q_HEADS // ATT_KV_HEADS
ATT_HEAD_DIM = 64
ATT_Q_WIDTH = ATT_Q_HEADS * ATT_HEAD_DIM
ATT_KV_WIDTH = ATT_KV_HEADS * ATT_HEAD_DIM
WINDOW = 128
ATT_BLOCK = 128
PROJ_SIZES = (HG_QK_WIDTH, HG_QK_WIDTH, HG_V_WIDTH, HG_V_WIDTH,
              ATT_Q_WIDTH, ATT_KV_WIDTH, ATT_KV_WIDTH, D_MODEL, D_MODEL)
PROJ_WIDTH = sum(PROJ_SIZES)
N_GROUPS = 4
EXPERTS_PER_GROUP = 8
N_EXPERTS = N_GROUPS * EXPERTS_PER_GROUP
TOP_K_IN_GROUP = 2
D_EXPERT = 512
MOE_BLOCK = 256
DN_ALPHA = (2.0 * DEPTH) ** 0.25
DN_BETA = (8.0 * DEPTH) ** -0.25
LN_EPS = 1e-5
RMS_EPS = 1e-6
NEG_INF = -1e30

kernel_name = "hybrid_hgrn2_swa_hiermoe_deepnorm"


def layer_norm(x, g, b):
    xf = x.astype(jnp.float32)
    mu = jnp.mean(xf, axis=-1, keepdims=True)
    var = jnp.mean(jnp.square(xf - mu), axis=-1, keepdims=True)
    return ((xf - mu) * lax.rsqrt(var + LN_EPS) * g.astype(jnp.float32) + b.astype(jnp.float32)).astype(x.dtype)


def hgrn2(q, f_logit, inp, g, lb, norm_w):
    B, S, _ = q.shape
    nc = S // HG_CHUNK
    f32 = jnp.float32
    qf = jax.nn.silu(q.astype(f32))
    f = lb + (1.0 - lb) * jax.nn.sigmoid(f_logit.astype(f32))
    logf = jnp.log(f)
    k = 1.0 - f

    def to_chunks(t, d):
        return t.reshape(B, nc, HG_CHUNK, HG_HEADS, d).transpose(1, 0, 3, 2, 4)

    qc = to_chunks(qf, HG_DK)
    kc = to_chunks(k, HG_DK)
    lc = to_chunks(logf, HG_DK)
    vc = to_chunks(inp.astype(f32), HG_DV)
    tri = jnp.tril(jnp.ones((HG_CHUNK, HG_CHUNK), dtype=bool))

    def step(state, xs):
        qb, kb, vb, lb_c = xs
        b = jnp.cumsum(lb_c, axis=2)
        o_inter = jnp.einsum('bhtk,bhkv->bhtv', qb * jnp.exp(b), state)
        diff = b[:, :, :, None, :] - b[:, :, None, :, :]
        decay = jnp.exp(jnp.where(tri[:, :, None], diff, -jnp.inf))
        scores = jnp.einsum('bhtk,bhtsk,bhsk->bhts', qb, decay, kb)
        o_intra = jnp.einsum('bhts,bhsv->bhtv', scores, vb)
        b_last = b[:, :, -1:, :]
        new_state = (jnp.exp(b_last[:, :, 0, :])[..., None] * state
                     + jnp.einsum('bhsk,bhsv->bhkv', kb * jnp.exp(b_last - b), vb))
        return new_state, o_inter + o_intra

    s0 = jnp.zeros((B, HG_HEADS, HG_DK, HG_DV), f32)
    _, o = lax.scan(step, s0, (qc, kc, vc, lc))
    o = o.transpose(1, 0, 3, 2, 4).reshape(B, S, HG_HEADS, HG_DV)
    o = o * lax.rsqrt(jnp.mean(jnp.square(o), axis=-1, keepdims=True) + RMS_EPS) * norm_w.astype(f32)
    o = o * jax.nn.silu(g.astype(f32).reshape(B, S, HG_HEADS, HG_DV))
    return o.reshape(B, S, HG_V_WIDTH).astype(q.dtype)


def sliding_window_attention(q, k, v, sinks):
    B, S, _ = q.shape
    nb = S // ATT_BLOCK
    f32 = jnp.float32
    qb = q.reshape(B, nb, ATT_BLOCK, ATT_KV_HEADS, ATT_GROUP, ATT_HEAD_DIM)
    pad = ((0, 0), (ATT_BLOCK, 0), (0, 0))
    kp = jnp.pad(k, pad).reshape(B, nb + 1, ATT_BLOCK, ATT_KV_HEADS, ATT_HEAD_DIM)
    vp = jnp.pad(v, pad).reshape(B, nb + 1, ATT_BLOCK, ATT_KV_HEADS, ATT_HEAD_DIM)
    kb = jnp.concatenate([kp[:, :-1], kp[:, 1:]], axis=2)
    vb = jnp.concatenate([vp[:, :-1], vp[:, 1:]], axis=2)
    scale = ATT_HEAD_DIM ** -0.5
    scores = jnp.einsum('bnqhgd,bnkhd->bnhgqk', qb, kb).astype(f32) * scale
    dist = (jnp.arange(ATT_BLOCK)[:, None] + ATT_BLOCK - jnp.arange(2 * ATT_BLOCK)[None, :])
    key_pos = jnp.arange(nb)[:, None] * ATT_BLOCK - ATT_BLOCK + jnp.arange(2 * ATT_BLOCK)[None, :]
    valid = ((dist >= 0) & (dist < WINDOW))[None] & (key_pos >= 0)[:, None, :]
    slopes = jnp.exp2(-8.0 * (jnp.arange(ATT_Q_HEADS, dtype=f32) + 1.0) / ATT_Q_HEADS)
    alibi = slopes.reshape(ATT_KV_HEADS, ATT_GROUP, 1, 1) * dist.astype(f32)
    logits = jnp.where(valid[None, :, None, None], scores - alibi, NEG_INF)
    sink_col = jnp.broadcast_to(sinks.astype(f32).reshape(ATT_KV_HEADS, ATT_GROUP, 1, 1),
                                logits.shape[:-1] + (1,))
    probs = jax.nn.softmax(jnp.concatenate([logits, sink_col], axis=-1), axis=-1)[..., :-1]
    out = jnp.einsum('bnhgqk,bnkhd->bnqhgd', probs.astype(v.dtype), vb)
    return out.reshape(B, S, ATT_Q_WIDTH)


def hierarchical_moe(x, w_rg, b_rg, w_re, b_re, w_gate, w_up, w_down):
    B, S, D = x.shape
    T = B * S
    A = T * TOP_K_IN_GROUP
    P = A + N_EXPERTS * MOE_BLOCK
    NB = P // MOE_BLOCK
    f32 = jnp.float32
    xt = x.reshape(T, D)
    glog = (xt @ w_rg + b_rg).astype(f32)
    gprob = jax.nn.softmax(glog, axis=-1)
    gval, gsel = lax.top_k(glog, 1)
    gw = jnp.take_along_axis(gprob, gsel, axis=-1)
    elog = (xt @ w_re + b_re).astype(f32).reshape(T, N_GROUPS, EXPERTS_PER_GROUP)
    elog_sel = jnp.take_along_axis(elog, gsel[:, :, None], axis=1)[:, 0]
    top_v, top_i = lax.top_k(elog_sel, TOP_K_IN_GROUP)
    weights = gw * jax.nn.softmax(top_v, axis=-1)
    expert_id = (gsel * EXPERTS_PER_GROUP + top_i).astype(jnp.int32)

    flat_e = expert_id.reshape(-1)
    flat_tok = jnp.repeat(jnp.arange(T, dtype=jnp.int32), TOP_K_IN_GROUP)
    flat_w = weights.reshape(-1)
    order = jnp.argsort(flat_e)
    se = flat_e[order]
    counts = jnp.bincount(flat_e, length=N_EXPERTS)
    padded = ((counts + MOE_BLOCK - 1) // MOE_BLOCK) * MOE_BLOCK
    start = jnp.cumsum(counts) - counts
    pend = jnp.cumsum(padded)
    pstart = pend - padded
    dest = pstart[se] + (jnp.arange(A) - start[se])
    slot_tok = jnp.zeros((P,), jnp.int32).at[dest].set(flat_tok[order])
    slot_w = jnp.zeros((P,), f32).at[dest].set(flat_w[order])
    block_expert = jnp.clip(jnp.searchsorted(pend, jnp.arange(NB) * MOE_BLOCK, side='right'),
                            0, N_EXPERTS - 1)
    xbuf = xt[slot_tok].reshape(NB, MOE_BLOCK, D)

    def expert_block(args):
        xb, e = args
        h = jax.nn.silu(xb @ w_gate[e]) * (xb @ w_up[e])
        return h @ w_down[e]

    ybuf = lax.map(expert_block, (xbuf, block_expert)).reshape(P, D)
    y = jnp.zeros((T, D), x.dtype).at[slot_tok].add(ybuf * slot_w[:, None].astype(x.dtype))
    return y.reshape(B, S, D)


def setup_inputs(seed: int = 0) -> dict:
    key = jax.random.key(seed)
    ks = jax.random.split(key, 20)
    n = jax.random.normal
    f32 = jnp.float32
    L = DEPTH
    return {
        "x": n(ks[0], (BATCH, SEQ, D_MODEL), f32),
        "lb_logits": 0.1 * n(ks[1], (DEPTH + 1, HG_QK_WIDTH), f32),
        "w_in": n(ks[2], (L, D_MODEL, PROJ_WIDTH), f32) * D_MODEL ** -0.5,
        "hg_norm_w": 1.0 + 0.02 * n(ks[3], (L, HG_DV), f32),
        "sinks": 0.5 * n(ks[4], (L, ATT_Q_HEADS), f32),
        "w_branch_a": n(ks[5], (L, HG_V_WIDTH, D_MODEL), f32) * HG_V_WIDTH ** -0.5,
        "w_branch_b": n(ks[6], (L, ATT_Q_WIDTH, D_MODEL), f32) * ATT_Q_WIDTH ** -0.5,
        "w_out": n(ks[7], (L, D_MODEL, D_MODEL), f32) * (D_MODEL ** -0.5 * DN_BETA),
        "ln1_g": 1.0 + 0.02 * n(ks[8], (L, D_MODEL), f32),
        "ln1_b": 0.02 * n(ks[9], (L, D_MODEL), f32),
        "router_group_w": n(ks[10], (L, D_MODEL, N_GROUPS), f32) * D_MODEL ** -0.5,
        "router_group_b": 0.01 * n(ks[11], (L, N_GROUPS), f32),
        "router_expert_w": n(ks[12], (L, D_MODEL, N_EXPERTS), f32) * D_MODEL ** -0.5,
        "router_expert_b": 0.01 * n(ks[13], (L, N_EXPERTS), f32),
        "w_exp_gate": n(ks[14], (L, N_EXPERTS, D_MODEL, D_EXPERT), f32) * D_MODEL ** -0.5,
        "w_exp_up": n(ks[15], (L, N_EXPERTS, D_MODEL, D_EXPERT), f32) * D_MODEL ** -0.5,
        "w_exp_down": n(ks[16], (L, N_EXPERTS, D_EXPERT, D_MODEL), f32) * (D_EXPERT ** -0.5 * DN_BETA),
        "ln2_g": 1.0 + 0.02 * n(ks[17], (L, D_MODEL), f32),
        "ln2_b": 0.02 * n(ks[18], (L, D_MODEL), f32),
    }


def reference(x, lb_logits, w_in, hg_norm_w, sinks, w_branch_a, w_branch_b, w_out,
              ln1_g, ln1_b, router_group_w, router_group_b, router_expert_w, router_expert_b,
              w_exp_gate, w_exp_up, w_exp_down, ln2_g, ln2_b):
    split_idx = list(np.cumsum(PROJ_SIZES)[:-1])
    lb_all = jnp.cumsum(jax.nn.softmax(lb_logits.astype(jnp.float32), axis=0), axis=0)
    for l in range(DEPTH):
        proj = x @ w_in[l]
        hq, hf, hi, hg, aq, ak, av, ga, gb = jnp.split(proj, split_idx, axis=-1)
        y_a = hgrn2(hq, hf, hi, hg, lb_all[l], hg_norm_w[l]) @ w_branch_a[l]
        y_b = sliding_window_attention(aq, ak, av, sinks[l]) @ w_branch_b[l]
        merged = jax.nn.sigmoid(ga) * y_a + jax.nn.sigmoid(gb) * y_b
        x = layer_norm(DN_ALPHA * x + merged @ w_out[l], ln1_g[l], ln1_b[l])
        moe = hierarchical_moe(x, router_group_w[l], router_group_b[l], router_expert_w[l],
                               router_expert_b[l], w_exp_gate[l], w_exp_up[l], w_exp_down[l])
        x = layer_norm(DN_ALPHA * x + moe, ln2_g[l], ln2_b[l])
    return x
```

```python
import contextlib
import numpy as np
import concourse.bass as bass
import concourse.mybir as mybir
from concourse.bass_utils import run_bass_kernel_spmd

F32 = mybir.dt.float32
BF16 = mybir.dt.bfloat16
I32 = mybir.dt.int32
AF = mybir.ActivationFunctionType
ALU = mybir.AluOpType
AX = mybir.AxisListType

N_CORES = 8
D = 1024
PROJ_W = 4864
NE = 32
DE = 512
CAP = 768
NSLOT = NE * CAP
ALPHA = 2.0 ** 0.25
LN_EPS = 1e-5
RMS_EPS = 1e-6
TB = 256
BIG = 1.0e30


class Tok:
    __slots__ = ("name", "w", "r", "dsem", "dcount")

    def __init__(self, name):
        self.name = name
        self.w = None
        self.r = {}
        self.dsem = None
        self.dcount = 0


class Sched:
    ENG = ("pe", "act", "dve", "pool", "sp")

    def __init__(self, nc, stack, n_dma_sems=90):
        self.nc = nc
        self.stack = stack
        self.sem_stack = stack
        self.prog = {e: [] for e in self.ENG}
        self.cnt = {e: 0 for e in self.ENG}
        self.seen = {e: {} for e in self.ENG}
        self.esem = {e: stack.enter_context(nc.semaphore("es_" + e)) for e in self.ENG}
        self.dsems = []
        self.n_dma_sems = n_dma_sems
        self.dma_toks = []
        self.nbuf = 0
        self.defer = None
        self.pending = []
        self.in_pump = False
        self.pump_ctr = 0

    def sb(self, name, shape, dtype):
        return self.stack.enter_context(self.nc.sbuf_tensor("s_" + name, list(shape), dtype))

    def ps(self, name, shape, dtype):
        return self.stack.enter_context(self.nc.psum_tensor("p_" + name, list(shape), dtype))

    def tok(self, name=None):
        self.nbuf += 1
        return Tok(name or "t%d" % self.nbuf)

    def _sem_of(self, key):
        if isinstance(key, str):
            return self.esem[key]
        return self.dsems[key[1]]

    def _deps(self, engine, reads, writes):
        deps = {}

        def add(k, v):
            if deps.get(k, 0) < v:
                deps[k] = v
        for t in reads:
            if t.w is not None:
                add(*t.w)
        for t in writes:
            if t.w is not None:
                add(*t.w)
            for k, v in t.r.items():
                add(k, v)
        out = []
        seen = self.seen[engine]
        for k, v in deps.items():
            if k == engine and engine == "pe":
                continue
            if seen.get(k, 0) >= v:
                continue
            seen[k] = v
            out.append((self._sem_of(k), v))
        return out

    def _pump(self, n=1):
        if self.in_pump or not self.pending:
            return
        self.in_pump = True
        for _ in range(n):
            if not self.pending:
                break
            kind, a = self.pending.pop(0)
            (self.op if kind == "op" else self.dma)(*a)
        self.in_pump = False

    def drain(self):
        self._pump(len(self.pending) + 1)

    def op(self, engine, fn, reads=(), writes=()):
        if self.defer is not None:
            self.defer.append(("op", (engine, fn, list(reads), list(writes))))
            return
        self._op(engine, fn, reads, writes)
        self.pump_ctr += 1
        if self.pump_ctr % 2 == 0:
            self._pump()

    def dma(self, queue, fn, reads=(), writes=(), chan=None):
        if self.defer is not None:
            self.defer.append(("dma", (queue, fn, list(reads), list(writes), chan)))
            return
        self._dma(queue, fn, reads, writes, chan)
        self._pump()

    def _op(self, engine, fn, reads=(), writes=()):
        waits = self._deps(engine, reads, writes)
        idx = self.cnt[engine] + 1
        self.cnt[engine] = idx
        sem = self.esem[engine]

        def emit(eng, waits=waits, fn=fn, sem=sem):
            for s, v in waits:
                eng.wait_ge(s, v)
            fn(eng).then_inc(sem, 1)
        self.prog[engine].append(emit)
        for t in reads:
            if t.r.get(engine, 0) < idx:
                t.r[engine] = idx
        for t in writes:
            t.w = (engine, idx)
            t.r = {}
        return idx

    def _dma(self, queue, fn, reads=(), writes=(), chan=None):
        waits = self._deps(queue, reads, writes)
        if chan.dsem is None:
            assert len(self.dsems) < self.n_dma_sems, "out of dma sems"
            chan.dsem = len(self.dsems)
            self.dsems.append(self.sem_stack.enter_context(self.nc.semaphore("ds_%d" % chan.dsem)))
            self.dma_toks.append(chan)
        chan.dcount += 1
        key = ("d", chan.dsem)
        val = 16 * chan.dcount
        sem = self.dsems[chan.dsem]

        def emit(eng, waits=waits, fn=fn, sem=sem):
            for s, v in waits:
                eng.wait_ge(s, v)
            fn(eng).then_inc(sem, 16)
        self.prog[queue].append(emit)
        for t in reads:
            if t.r.get(key, 0) < val:
                t.r[key] = val
        for t in writes:
            t.w = (key, val)
            t.r = {}

    def barrier(self):
        self.drain()
        targets = [(e, self.cnt[e]) for e in self.ENG if self.cnt[e] > 0]
        dtargets = [(("d", t.dsem), 16 * t.dcount) for t in self.dma_toks]
        for eng in self.ENG:
            waits = []
            seen = self.seen[eng]
            for k, v in targets + dtargets:
                if k == eng:
                    continue
                if seen.get(k, 0) >= v:
                    continue
                seen[k] = v
                waits.append((self._sem_of(k), v))

            def emit(e, waits=waits):
                for s, v in waits:
                    e.wait_ge(s, v)
            self.prog[eng].append(emit)

    def emit_all(self):
        nc = self.nc
        with nc.Block() as block:
            @block.tensor
            def _(e):
                for f in self.prog["pe"]:
                    f(e)

            @block.scalar
            def _(e):
                for f in self.prog["act"]:
                    f(e)

            @block.vector
            def _(e):
                for f in self.prog["dve"]:
                    f(e)

            @block.gpsimd
            def _(e):
                for f in self.prog["pool"]:
                    f(e)

            @block.sync
            def _(e):
                for f in self.prog["sp"]:
                    f(e)
        self.prog = {e: [] for e in self.ENG}


def MM(S, out, lhsT, rhs, start, stop, reads, writes):
    S.op("pe", lambda e: e.matmul(out, lhsT=lhsT, rhs=rhs, start=start, stop=stop), reads, writes)


def TR(S, out, in_, ident, reads, writes):
    S.op("pe", lambda e: e.transpose(out=out, in_=in_, identity=ident), reads, writes)


def ACT(S, out, in_, func, reads, writes, bias=None, scale=None, accum=None):
    kw = {}
    if bias is not None:
        kw["bias"] = bias
    if scale is not None:
        kw["scale"] = scale
    if accum is not None:
        kw["accum_out"] = accum
    S.op("act", lambda e: e.activation(out=out, in_=in_, func=func, **kw), reads, writes)


def TS(S, eng, out, in0, s1, s2, op0, op1, reads, writes):
    if op1 is None:
        S.op(eng, lambda e: e.tensor_scalar(out=out, in0=in0, scalar1=s1, scalar2=None, op0=op0), reads, writes)
    else:
        S.op(eng, lambda e: e.tensor_scalar(out=out, in0=in0, scalar1=s1, scalar2=s2, op0=op0, op1=op1), reads, writes)


def TT(S, eng, out, in0, in1, op, reads, writes):
    S.op(eng, lambda e: e.tensor_tensor(out=out, in0=in0, in1=in1, op=op), reads, writes)


def STT(S, out, in0, scalar, in1, op0, op1, reads, writes):
    S.op("dve", lambda e: e.scalar_tensor_tensor(out=out, in0=in0, scalar=scalar, in1=in1, op0=op0, op1=op1), reads, writes)


def CP(S, eng, out, in_, reads, writes):
    if eng == "act":
        S.op("act", lambda e: e.copy(out=out, in_=in_), reads, writes)
    else:
        S.op(eng, lambda e: e.tensor_copy(out=out, in_=in_), reads, writes)


def LOAD(S, queue, out, in_, tok, extra_reads=()):
    S.dma(queue, lambda e: e.dma_start(out=out, in_=in_), reads=list(extra_reads), writes=[tok], chan=tok)


_BC = {}


def _bc_reg(e, phase):
    if _BC.get("nc") is not e:
        _BC.clear()
        _BC["nc"] = e
        _BC["reg"] = e.alloc_register("bc")
    if _BC.get("phase") != phase:
        e.reg_mov(_BC["reg"], NSLOT - 1)
        _BC["phase"] = phase
    return _BC["reg"]


def build(NSEQ=4, SEQ=2048, DBG=None):
    NTOK = NSEQ * SEQ
    NSB = SEQ // TB
    NTILE = NTOK // 128
    nc = bass.Bass("TRN2", target_bir_lowering=False)

    def din(name, shape, dtype=F32):
        return nc.dram_tensor(name, list(shape), dtype, kind="ExternalInput").ap()

    x_d = din("x", [NTOK, D])
    win_d = din("w_in", [D, PROJ_W])
    lbt_d = din("lbt", [128, 8])
    nw_d = din("nw", [128, 1])
    wa_d = din("w_a", [512, D])
    wb_d = din("w_b", [512, D])
    wo_d = din("w_o", [D, D])
    ln1g_d = din("ln1g", [128, D])
    ln1b_d = din("ln1b", [128, D])
    ln2g_d = din("ln2g", [128, D])
    ln2b_d = din("ln2b", [128, D])
    wr_d = din("wr", [D, 36])
    rb_d = din("rb", [128, 36])
    wg_d = din("wg", [NE, D, DE])
    wu_d = din("wu", [NE, D, DE])
    wd_d = din("wd", [NE, DE, D])
    ident_d = din("ident", [128, 128])
    bdm_d = din("bdmask", [128, 128])
    rmask_d = din("rmask", [128, TB])
    abias_d = din("abias", [128, 8 * 2 * 128], BF16)
    sinkr_d = din("sinkr", [1, 512])
    utri_d = din("utri", [128, 128])
    sbase_d = din("sbase", [128, NE])
    out_d = nc.dram_tensor("out", [NTOK, D], F32, kind="ExternalOutput").ap()
    x1a_d = nc.dram_tensor("x1a", [NTOK, D], F32, kind="Internal").ap()
    xe_d = nc.dram_tensor("xe", [NSLOT, D], BF16, kind="Internal").ap()
    yb_d = nc.dram_tensor("yb", [NSLOT, D], BF16, kind="Internal").ap()

    with contextlib.ExitStack() as st:
        S = Sched(nc, st)
        PA = S.ps("PA", [128, 1024], F32); tPA = [S.tok(), S.tok()]
        P1 = S.ps("P1", [128, 512], F32); tP1 = S.tok()
        P2 = S.ps("P2", [128, 1024], BF16); tP2 = S.tok()
        P45 = S.ps("P45", [128, 1024], F32); tP45 = [S.tok(), S.tok()]
        P6 = S.ps("P6", [128, 512], F32); tP6 = S.tok()
        P37 = S.ps("P37", [128, 512], F32); tP37 = S.tok()
        ident_f = S.sb("ident_f", [128, 128], F32); t_idf = S.tok()
        ident_b = S.sb("ident_b", [128, 128], BF16); t_idb = S.tok()
        slot_i = S.sb("slot_i", [128, NTILE, 2], I32)
        slot_w = S.sb("slot_w", [128, NTILE, 2], F32)
        t_slot = [S.tok() for _ in range(NTILE)]
        LOAD(S, "sp", ident_f[:], ident_d, t_idf)
        CP(S, "dve", ident_b[:], ident_f[:], [t_idf], [t_idb])

        with contextlib.ExitStack() as st1:
            S.stack = st1
            win = S.sb("win", [128, 8, PROJ_W], BF16); t_win = S.tok()
            wa = S.sb("wa", [128, 4, D], BF16); t_wa = S.tok()
            wb = S.sb("wb", [128, 4, D], BF16); t_wb = S.tok()
            wo = S.sb("wo", [128, 8, D], BF16); t_wo = S.tok()
            win_v = win_d.rearrange("(kc p) c -> p kc c", p=128)
            for c4 in range(4):
                S.dma("pool", (lambda a, b: (lambda e: e.dma_start(out=a, in_=b)))(
                    win[:, :, c4 * 1216:(c4 + 1) * 1216], win_v[:, :, c4 * 1216:(c4 + 1) * 1216]),
                    writes=[t_win], chan=t_win)
            LOAD(S, "pool", wa[:], wa_d.rearrange("(kc p) c -> p kc c", p=128), t_wa)
            LOAD(S, "pool", wb[:], wb_d.rearrange("(kc p) c -> p kc c", p=128), t_wb)
            LOAD(S, "pool", wo[:], wo_d.rearrange("(kc p) c -> p kc c", p=128), t_wo)
            bdm = S.sb("bdm", [128, 128], F32); t_bdm = S.tok()
            rmask = S.sb("rmask", [128, TB], F32); t_rmask = S.tok()
            abias = S.sb("abias", [128, 8 * 2 * 128], BF16); t_abias = S.tok()
            utri_f = S.sb("utri_f", [128, 128], F32); t_utf = S.tok()
            utri = S.sb("utri", [128, 128], BF16); t_utri = S.tok()
            ones_b = S.sb("ones_b", [128, 128], BF16); t_ones = S.tok()
            onesv = S.sb("onesv", [128, 128], BF16); t_onesv = S.tok()
            sbase = S.sb("sbase", [128, NE], F32); t_sbase = S.tok()
            lbt = S.sb("lbt", [128, 8], F32); t_lbt = S.tok()
            lbc = S.sb("lbc", [128, 4], F32); t_lbc = S.tok()
            oml = S.sb("oml", [128, 4], F32); t_oml = S.tok()
            nw = S.sb("nw", [128, 1], F32); t_nw = S.tok()
            ln1g = S.sb("ln1g", [128, D], F32); t_ln1g = S.tok()
            ln1b = S.sb("ln1b", [128, D], F32); t_ln1b = S.tok()
            wr = S.sb("wr", [128, 8, 36], F32); t_wr = S.tok()
            rb = S.sb("rb", [128, 36], F32); t_rb = S.tok()
            tot = S.sb("tot", [128, NE], F32); t_tot = S.tok()
            LOAD(S, "sp", bdm[:], bdm_d, t_bdm)
            LOAD(S, "sp", rmask[:], rmask_d, t_rmask)
            LOAD(S, "sp", abias[:], abias_d, t_abias)
            LOAD(S, "sp", utri_f[:], utri_d, t_utf)
            LOAD(S, "sp", sbase[:], sbase_d, t_sbase)
            LOAD(S, "sp", lbt[:], lbt_d, t_lbt)
            LOAD(S, "sp", nw[:], nw_d, t_nw)
            LOAD(S, "sp", ln1g[:], ln1g_d, t_ln1g)
            LOAD(S, "sp", ln1b[:], ln1b_d, t_ln1b)
            LOAD(S, "sp", wr[:], wr_d.rearrange("(kc p) c -> p kc c", p=128), t_wr)
            LOAD(S, "sp", rb[:], rb_d, t_rb)
            CP(S, "dve", utri[:], utri_f[:], [t_utf], [t_utri])
            S.op("dve", lambda e: e.memset(ones_b[:], 1.0), [], [t_ones])
            S.op("dve", lambda e: e.memset(onesv[:], 1.0 / 128.0), [], [t_onesv])
            S.op("dve", lambda e: e.memset(tot[:], 0.0), [], [t_tot])
            TT(S, "dve", lbc[:], lbt[:, 0:4], lbt[:, 4:8], ALU.subtract, [t_lbt], [t_lbc])
            ACT(S, lbc[:], lbc[:], AF.Sigmoid, [t_lbc], [t_lbc])
            TS(S, "dve", oml[:], lbc[:], -1.0, 1.0, ALU.mult, ALU.add, [t_lbc], [t_oml])
            xs = S.sb("xs", [128, 2, 2, D], F32)
            t_xs = [[S.tok(), S.tok()], [S.tok(), S.tok()]]
            xb = S.sb("xb", [128, D], BF16); t_xb = S.tok()
            xT = S.sb("xT", [128, 8, TB], BF16); t_xT = [S.tok(), S.tok()]
            qs = S.sb("qs", [128, TB], F32); t_qs = S.tok()
            ebt = S.sb("ebt", [128, TB], F32); t_ebt = S.tok()
            ebl = S.sb("ebl", [128, 4, 4], F32); t_ebl = [S.tok() for _ in range(4)]
            tmp1 = S.sb("tmp1", [128, TB], F32); t_tmp1 = S.tok()
            tmp2 = S.sb("tmp2", [128, TB], F32); t_tmp2 = S.tok()
            tmp3 = S.sb("tmp3", [128, TB], F32); t_tmp3 = S.tok()
            Qt = S.sb("Qt", [128, 4, TB], BF16); t_Qt = [S.tok() for _ in range(4)]
            Kt = S.sb("Kt", [128, 4, TB], BF16); t_Kt = [S.tok() for _ in range(4)]
            Kh = S.sb("Kh", [128, 4, TB], BF16); t_Kh = [S.tok() for _ in range(4)]
            gs = S.sb("gs", [128, 4, TB], BF16); t_gs = [S.tok() for _ in range(4)]
            aqT = S.sb("aqT", [128, 4, TB], BF16); t_aq = [S.tok() for _ in range(4)]
            kT = S.sb("kT", [128, 128 + TB], BF16); t_kT = S.tok(); t_kTp = S.tok()
            vt = S.sb("vt", [128, 2, 512], BF16); t_vt = [S.tok(), S.tok()]
            avt = S.sb("avt", [128, 3, 128], BF16); t_avt = [S.tok() for _ in range(3)]
            S_f = S.sb("S_f", [128, 4, 128], F32); t_Sf = [S.tok() for _ in range(4)]
            S_b = S.sb("S_b", [128, 4, 128], BF16); t_Sb = [S.tok() for _ in range(4)]
            sm = S.sb("sm", [128, 4, 128], BF16); t_sm = [S.tok() for _ in range(4)]
            khtok = S.sb("khtok", [128, 4, 128], BF16); t_kht = [S.tok() for _ in range(4)]
            bdm4 = S.sb("bdm4", [128, 512], BF16); t_bdm4 = S.tok()
            for h in range(4):
                CP(S, "dve", bdm4[:, h * 128:(h + 1) * 128], bdm[:], [t_bdm], [t_bdm4])
            o2 = S.sb("o2", [128, 2, TB], BF16); t_o2 = [S.tok(), S.tok()]
            ofin = S.sb("ofin", [128, 4, TB], BF16); t_ofin = [S.tok() for _ in range(4)]
            lg = S.sb("lg", [128, 1024], F32); t_lg = S.tok()
            Pm = S.sb("Pm", [128, 1024], BF16); t_Pm = S.tok()
            rr = S.sb("rr", [128, 512], F32); t_rr = S.tok()
            esr = S.sb("esr", [1, 512], F32); t_esr = S.tok()
            ones_r = S.sb("ones_r", [1, 256], F32); t_onesr = S.tok()
            LOAD(S, "sp", esr[:], sinkr_d, t_esr)
            ACT(S, esr[:], esr[:], AF.Exp, [t_esr], [t_esr])
            S.op("dve", lambda e: e.memset(ones_r[:], 1.0), [], [t_onesr])
            attT = S.sb("attT", [128, 4, TB], BF16); t_att = [S.tok() for _ in range(4)]
            mT = S.sb("mT", [128, 8, TB], BF16); t_mT = [S.tok() for _ in range(8)]
            mscr = S.sb("mscr", [128, 4, TB], F32)
            sga = mscr[:, 0, :]; t_sga = S.tok()
            sgb = mscr[:, 1, :]; t_sgb = S.tok()
            mt1 = mscr[:, 2, :]; t_mt1 = S.tok()
            mt2 = mscr[:, 3, :]; t_mt2 = S.tok()
            t_mscr = [t_sga, t_sgb, t_mt1, t_mt2]
            h1 = S.sb("h1", [128, D], F32); t_h1 = S.tok()
            o_f = h1[:, 0:512].rearrange("p (b c) -> p b c", c=TB); t_of = [S.tok(), S.tok()]
            rstd = h1[:, 512:1024].rearrange("p (b c) -> p b c", c=TB); t_rstd = [S.tok(), S.tok()]
            x1 = S.sb("x1", [128, D], F32); t_x1 = S.tok()
            x1_bufs = (x1[:], mT[:].rearrange("p a b -> p (a b)").bitcast(F32))
            x1_toks = ([t_x1], [S.tok()] + t_mT)
            x1al = h1; t_x1al = t_h1
            x1b = Pm; t_x1b = S.tok()
            x1T = mscr[:].rearrange("p a (b c) -> p (a b) c", c=128)
            stats = S.sb("stats", [128, 2, 6], F32); t_stats = S.tok()
            mv = S.sb("mv", [128, 2], F32); t_mv = S.tok()
            rs1 = S.sb("rs1", [128, 2], F32); t_rs1 = S.tok()
            lg36 = S.sb("lg36", [128, 36], F32); t_lg36 = S.tok()
            rt = S.sb("rt", [128, 16], F32); t_rt = S.tok()
            gmask = S.sb("gmask", [128, 4], F32); t_gmask = S.tok()
            gex = S.sb("gex", [128, 4], F32); t_gex = S.tok()
            elm = S.sb("elm", [128, 32], F32); t_elm = S.tok()
            m8 = S.sb("m8", [128, 8], F32); t_m8 = S.tok()
            mk1 = S.sb("mk1", [128, 32], F32); t_mk1 = S.tok()
            mk2 = S.sb("mk2", [128, 32], F32); t_mk2 = S.tok()
            mkb = S.sb("mkb", [128, 32], BF16); t_mkb = S.tok()
            posf = S.sb("posf", [128, 32], F32); t_posf = S.tok()
            vld = S.sb("vld", [128, 32], F32); t_vld = S.tok()
            scr32 = S.sb("scr32", [128, 32], F32); t_scr32 = S.tok()
            slf = S.sb("slf", [128, 2], F32); t_slf = S.tok()

            t_xez = S.tok()
            S.op("pool", lambda e: e.memset(Pm[:], 0.0), [], [t_Pm])
            if DBG is None:
                xe_v = xe_d.rearrange("(n p) d -> p n d", p=128)
                nrow = NSLOT // 128
                for r0 in range(0, nrow, 1):
                    S.dma("sp", (lambda a, b: (lambda e: e.dma_start(out=a, in_=b)))(
                        xe_v[:, r0:r0 + 1, :], Pm[:].rearrange("p (n d) -> p n d", d=D)),
                        reads=[t_Pm], writes=[t_xez], chan=t_xez)

            pj_state = [0]
            tokB = [S.tok() for _ in range(5)]
            esets = (
                (tmp1[:], tmp2[:], tmp3[:], qs[:], ebt[:], t_tmp1, t_tmp2, t_tmp3, t_qs, t_ebt, [], []),
                (lg[:, 0:256], lg[:, 256:512], lg[:, 512:768], lg[:, 768:1024], rr[:, 0:256],
                 tokB[0], tokB[1], tokB[2], tokB[3], tokB[4], [t_lg], [t_rr]),
            )
            po_loc = ((P45, 0), (P45, 512), (P6, 0), (PA, 256))
            po_tok = (tP45[0], tP45[1], tP6, tPA[0])

            def po_ap(h, c0, n):
                pt, off = po_loc[h]
                return pt[:, off + c0:off + c0 + n]

            def projF(col0, evac):
                u = pj_state[0] % 2
                pj_state[0] += 1
                pp = PA[:, u * 512:u * 512 + 256]
                for kc in range(8):
                    MM(S, pp, win[:, kc, col0:col0 + 128], xT[:, kc, :], kc == 0, kc == 7,
                       [t_win, t_xT[0], t_xT[1]], [tPA[u]])
                evac(pp, tPA[u])

            def load_x(sbi):
                par = sbi % 2
                g0 = sbi * TB
                for t in range(2):
                    LOAD(S, "sp", xs[:, par, t, :], x_d[g0 + t * 128:g0 + (t + 1) * 128, :], t_xs[par][t])

            load_x(0)
            for sbi in range(NSEQ * NSB):
                par = sbi % 2
                jb = sbi % NSB
                g0 = sbi * TB
                if jb == 0:
                    for h in range(4):
                        S.op("dve", (lambda a: (lambda e: e.memset(a, 0.0)))(S_f[:, h, :]), [], [t_Sf[h]])
                        S.op("pool", (lambda a: (lambda e: e.memset(a, 0.0)))(S_b[:, h, :]), [], [t_Sb[h]])
                for t in range(2):
                    CP(S, "act", xb[:], xs[:, par, t, :], [t_xs[par][t]], [t_xb])
                    for kc in range(8):
                        TR(S, P2[:, kc * 128:(kc + 1) * 128], xb[:, kc * 128:(kc + 1) * 128], ident_b[:],
                           [t_xb, t_idb], [tP2])
                    CP(S, "dve", xT[:, :, t * 128:(t + 1) * 128], P2[:].rearrange("p (k c) -> p k c", c=128),
                       [tP2], [t_xT[t]])
                for t in range(2):
                    u = pj_state[0] % 2
                    pj_state[0] += 1
                    for kc in range(8):
                        MM(S, PA[:, u * 512:(u + 1) * 512], xT[:, kc, t * 128:(t + 1) * 128], win[:, kc, 1024:1536],
                           kc == 0, kc == 7, [t_win, t_xT[t]], [tPA[u]])
                    CP(S, "act", vt[:, t, :], PA[:, u * 512:(u + 1) * 512], [tPA[u]], [t_vt[t]])
                    u = pj_state[0] % 2
                    pj_state[0] += 1
                    for kc in range(8):
                        MM(S, PA[:, u * 512:u * 512 + 128], xT[:, kc, t * 128:(t + 1) * 128], win[:, kc, 2688:2816],
                           kc == 0, kc == 7, [t_win, t_xT[t]], [tPA[u]])
                    CP(S, "dve", avt[:, 1 + t, :], PA[:, u * 512:u * 512 + 128], [tPA[u]], [t_avt[1 + t]])
                for j in range(4):
                    projF(2048 + j * 128, lambda pp, tk, j=j: CP(S, "act", aqT[:, j, :], pp, [tk], [t_aq[j]]))
                projF(2560, lambda pp, tk: CP(S, "dve", kT[:, 128:128 + TB], pp, [tk], [t_kT]))
                if DBG == "a":
                    break
                def el_stage1(h):
                    T1, T2, T3, Q_, E_, k1, k2, k3, kq, ke, xw, xe = esets[h % 2]
                    projF(0 + h * 128, lambda pp, tk: ACT(S, Q_, pp, AF.Silu, [tk], [kq] + xw))
                    projF(1536 + h * 128, lambda pp, tk: ACT(S, gs[:, h, :], pp, AF.Silu, [tk], [t_gs[h]]))
                    projF(512 + h * 128, lambda pp, tk: ACT(S, T1, pp, AF.Sigmoid, [tk], [k1] + xw))
                    TS(S, "dve", T3, T1, oml[:, h:h + 1], lbc[:, h:h + 1], ALU.mult, ALU.add,
                       [k1, t_oml, t_lbc], [k3] + xw)

                def el_stage2(h):
                    T1, T2, T3, Q_, E_, k1, k2, k3, kq, ke, xw, xe = esets[h % 2]
                    ACT(S, T2, T3, AF.Ln, [k3], [k2] + xw)
                    S.op("dve", (lambda o_, d1: (lambda e: e.tensor_tensor_scan(out=o_, data0=rmask[:], data1=d1, initial=0.0,
                                                                               op0=ALU.mult, op1=ALU.add)))(T1, T2),
                         [t_rmask, k2], [k1] + xw)
                    ACT(S, E_, T1, AF.Exp, [k1], [ke] + xe)
                    CP(S, "pool", ebl[:, h, :], E_[:, 63:TB:64], [ke], [t_ebl[h]])
                    ACT(S, T2, T1, AF.Exp, [k1], [k2] + xw, scale=-1.0)
                    TT(S, "dve", Qt[:, h, :], Q_, E_, ALU.mult, [kq, ke], [t_Qt[h]])
                    TS(S, "dve", Q_, T3, -1.0, 1.0, ALU.mult, ALU.add, [k3], [kq] + xw)
                    TT(S, "dve", Kt[:, h, :], Q_, T2, ALU.mult, [kq, k2], [t_Kt[h]])
                    for c in range(4):
                        TS(S, "dve", Kh[:, h, c * 64:(c + 1) * 64], Kt[:, h, c * 64:(c + 1) * 64],
                           ebl[:, h, c:c + 1], None, ALU.mult, None, [t_Kt[h], t_ebl[h]], [t_Kh[h]])

                el_stage1(0)
                el_stage1(1)
                el_stage2(0)
                el_stage1(2)
                el_stage2(1)
                el_stage1(3)
                el_stage2(2)
                el_stage2(3)
                if DBG == "b":
                    break
                S.drain()
                if sbi + 1 < NSEQ * NSB and DBG not in ("a", "b", "c", "d", "e"):
                    load_x(sbi + 1)
                for t in range(2):
                    tc0 = t * 128
                    for h in range(4):
                        MM(S, P37[:, h * 128:(h + 1) * 128], Kt[:, h, tc0:tc0 + 128], Qt[:, h, tc0:tc0 + 128], True, True,
                           [t_Kt[h], t_Qt[h]], [tP37])
                        TR(S, P2[:, h * 128:(h + 1) * 128], Kh[:, h, tc0:tc0 + 128], ident_b[:], [t_Kh[h], t_idb], [tP2])
                    TT(S, "dve", sm[:].rearrange("p h c -> p (h c)"), P37[:, :], bdm4[:], ALU.mult, [tP37, t_bdm4], t_sm)
                    CP(S, "act", khtok[:].rearrange("p h c -> p (h c)"), P2[:, 0:512], [tP2], t_kht)
                    for c in range(2):
                        cc0 = tc0 + c * 64
                        for h in range(4):
                            if c == 0:
                                MM(S, po_ap(h, tc0, 128), vt[:, t, h * 128:(h + 1) * 128], sm[:, h, :],
                                   True, False, [t_vt[t], t_sm[h]], [po_tok[h]])
                            MM(S, po_ap(h, cc0, 64), S_b[:, h, :], Qt[:, h, cc0:cc0 + 64],
                               False, c == 1, [t_Sb[h], t_Qt[h]], [po_tok[h]])
                            MM(S, P1[:, h * 128:(h + 1) * 128], khtok[c * 64:(c + 1) * 64, h, :],
                               vt[c * 64:(c + 1) * 64, t, h * 128:(h + 1) * 128], True, True, [t_kht[h], t_vt[t]], [tP1])
                        for h in range(4):
                            STT(S, S_f[:, h, :], S_f[:, h, :], ebl[:, h, t * 2 + c:t * 2 + c + 1], P1[:, h * 128:(h + 1) * 128],
                                ALU.mult, ALU.add, [t_Sf[h], t_ebl[h], tP1], [t_Sf[h]])
                        CP(S, "act", S_b[:].rearrange("p h c -> p (h c)"), S_f[:].rearrange("p h c -> p (h c)"), t_Sf, t_Sb)
                for h in range(4):
                    bb = h % 2
                    ACT(S, o2[:, bb, :], po_ap(h, 0, TB), AF.Square, [po_tok[h]], [t_o2[bb]])
                    pms = PA[:, 512:512 + 256]
                    MM(S, pms, onesv[:], o2[:, bb, :], True, True, [t_onesv, t_o2[bb]], [tPA[1]])
                    ACT(S, rstd[:, bb, :], pms, AF.Ln, [tPA[1]], [t_rstd[bb], t_h1], bias=RMS_EPS)
                    ACT(S, rstd[:, bb, :], rstd[:, bb, :], AF.Exp, [t_rstd[bb]], [t_rstd[bb]], scale=-0.5)
                    STT(S, o_f[:, bb, :], po_ap(h, 0, TB), nw[:, 0:1], rstd[:, bb, :], ALU.mult, ALU.mult,
                        [po_tok[h], t_nw, t_rstd[bb]], [t_of[bb], t_h1])
                    TT(S, "dve", ofin[:, h, :], o_f[:, bb, :], gs[:, h, :], ALU.mult, [t_of[bb], t_gs[h]], [t_ofin[h]])
                if DBG == "c":
                    break
                items = [(t, jp) for t in range(2) for jp in range(2)]

                def att_scores(t, jp):
                    tc0 = t * 128
                    has_prev = not (jb == 0 and t == 0)
                    pcs = (0, 1) if has_prev else (1,)
                    for jl in range(2):
                        j = jp * 2 + jl
                        for pc in pcs:
                            kc0 = tc0 + pc * 128
                            col = (jl * 2 + pc) * 128
                            for hh in range(2):
                                r0 = hh * 64
                                MM(S, (P1, P37)[hh][:, col:col + 128], kT[r0:r0 + 64, kc0:kc0 + 128],
                                   aqT[r0:r0 + 64, j, tc0:tc0 + 128], True, True,
                                   [t_kT, t_kTp, t_aq[j]], [(tP1, tP37)[hh]])

                att_scores(*items[0])
                for it, (t, jp) in enumerate(items):
                    tc0 = t * 128
                    has_prev = not (jb == 0 and t == 0)
                    pcs = (0, 1) if has_prev else (1,)
                    lg5 = lg[:].rearrange("p (a h c) -> p a h c", h=2, c=128)
                    for hh in range(2):
                        bank = (P1, P37)[hh]
                        tb_ = (tP1, tP37)[hh]
                        ab0 = (hh * 4 + jp * 2) * 256
                        if has_prev:
                            STT(S, lg5[:, :, hh, :], bank[:, :].rearrange("p (a c) -> p a c", c=128), 0.125,
                                abias[:, ab0:ab0 + 512].rearrange("p (a c) -> p a c", c=128),
                                ALU.mult, ALU.add, [tb_, t_abias], [t_lg] + tokB[0:4])
                        else:
                            for jl in range(2):
                                a_i = jl * 2 + 1
                                STT(S, lg5[:, a_i, hh, :], bank[:, a_i * 128:(a_i + 1) * 128], 0.125,
                                    abias[:, ab0 + a_i * 128:ab0 + (a_i + 1) * 128],
                                    ALU.mult, ALU.add, [tb_, t_abias], [t_lg] + tokB[0:4])
                    if has_prev:
                        ACT(S, Pm[:], lg[:], AF.Exp, [t_lg], [t_Pm, t_x1b])
                    else:
                        ACT(S, Pm[:].rearrange("p (j q c) -> p j q c", q=2, c=256)[:, :, 1, :],
                            lg[:].rearrange("p (j q c) -> p j q c", q=2, c=256)[:, :, 1, :], AF.Exp, [t_lg], [t_Pm, t_x1b])
                    if it + 1 < len(items):
                        att_scores(*items[it + 1])
                    for jl in range(2):
                        for k_i, pc in enumerate(pcs):
                            seg = Pm[:, (jl * 2 + pc) * 256:(jl * 2 + pc + 1) * 256]
                            MM(S, P6[:, jl * 256:(jl + 1) * 256], avt[:, t + pc, :], seg, k_i == 0, k_i == len(pcs) - 1,
                               [t_avt[t + pc], t_Pm], [tP6])
                    for jl in range(2):
                        j = jp * 2 + jl
                        for k_i, pc in enumerate(pcs):
                            seg = Pm[:, (jl * 2 + pc) * 256:(jl * 2 + pc + 1) * 256]
                            MM(S, PA[:, jl * 256:(jl + 1) * 256], ones_b[:], seg, k_i == 0, False,
                               [t_ones, t_Pm], [tPA[0]])
                        MM(S, PA[:, jl * 256:(jl + 1) * 256], esr[0:1, j * 128:(j + 1) * 128], ones_r[0:1, :], False, True,
                           [t_esr, t_onesr], [tPA[0]])
                    ACT(S, rr[:], PA[:, 0:512], AF.Ln, [tPA[0]], [t_rr, tokB[4]])
                    ACT(S, rr[:], rr[:], AF.Exp, [t_rr], [t_rr], scale=-1.0)
                    for hh in range(2):
                        r0 = hh * 64
                        TT(S, "dve", attT[r0:r0 + 64, jp * 2:jp * 2 + 2, tc0:tc0 + 128],
                           P6[r0:r0 + 64, :].rearrange("p (j c) -> p j c", c=256)[:, :, hh * 128:(hh + 1) * 128],
                           rr[r0:r0 + 64, :].rearrange("p (j c) -> p j c", c=256)[:, :, hh * 128:(hh + 1) * 128],
                           ALU.mult, [tP6, t_rr], [t_att[jp * 2], t_att[jp * 2 + 1]])
                CP(S, "pool", kT[:, 0:128], kT[:, TB:TB + 128], [t_kT], [t_kTp])
                CP(S, "pool", avt[:, 0, :], avt[:, 2, :], [t_avt[2]], [t_avt[0]])
                if DBG == "d":
                    break
                for fc in range(8):
                    fcs = slice(fc * 128, (fc + 1) * 128)
                    bank = (P6, P1)[fc % 2]
                    tb_ = (tP6, tP1)[fc % 2]
                    for h in range(4):
                        MM(S, bank[:, 0:256], wa[:, h, fcs], ofin[:, h, :], h == 0, h == 3, [t_wa, t_ofin[h]], [tb_])
                    for j in range(4):
                        MM(S, bank[:, 256:512], wb[:, j, fcs], attT[:, j, :], j == 0, j == 3, [t_wb, t_att[j]], [tb_])
                    projF(2816 + fc * 128, lambda pp, tk: ACT(S, sga, pp, AF.Sigmoid, [tk], [t_sga]))
                    projF(3840 + fc * 128, lambda pp, tk: ACT(S, sgb, pp, AF.Sigmoid, [tk], [t_sgb]))
                    TT(S, "dve", mt1, bank[:, 0:256], sga, ALU.mult, [tb_, t_sga], [t_mt1])
                    TT(S, "dve", mt2, bank[:, 256:512], sgb, ALU.mult, [tb_, t_sgb], [t_mt2])
                    TT(S, "pool", mT[:, fc, :], mt1, mt2, ALU.add, [t_mt1, t_mt2], [t_mT[fc]])
                if DBG == "e":
                    break
                op_loc = ((P45[:, 0:512], P45[:, 512:1024], tP45[0], tP45[1]), (P6[:, :], P1[:, :], tP6, tP1))
                for t in range(2):
                    tc0 = t * 128
                    for half in range(2):
                        for fc in range(8):
                            MM(S, op_loc[t][half], mT[:, fc, tc0:tc0 + 128],
                               wo[:, fc, half * 512:(half + 1) * 512], fc == 0, fc == 7,
                               [t_mT[fc], t_wo], [op_loc[t][2 + half]])
                S.defer = []
                for t in range(2):
                    tile_i = sbi * 2 + t
                    tc0 = t * 128
                    tok0 = g0 + tc0
                    X1 = x1_bufs[t]
                    kx = x1_toks[t]
                    for half in range(2):
                        STT(S, h1[:, half * 512:(half + 1) * 512], xs[:, par, t, half * 512:(half + 1) * 512], ALPHA,
                            op_loc[t][half], ALU.mult, ALU.add, [t_xs[par][t], op_loc[t][2 + half]], [t_h1] + t_of + t_rstd)
                    for half in range(2):
                        S.op("dve", (lambda a, b: (lambda e: e.bn_stats(out=a, in_=b)))(
                            stats[:, half, :], h1[:, half * 512:(half + 1) * 512]), [t_h1], [t_stats])
                    S.op("dve", lambda e: e.bn_aggr(out=mv[:], in_=stats[:].rearrange("p a b -> p (a b)")), [t_stats], [t_mv])
                    ACT(S, rs1[:, 0:1], mv[:, 1:2], AF.Ln, [t_mv], [t_rs1], bias=LN_EPS)
                    ACT(S, rs1[:, 0:1], rs1[:, 0:1], AF.Exp, [t_rs1], [t_rs1], scale=-0.5)
                    STT(S, rs1[:, 1:2], mv[:, 0:1], -1.0, rs1[:, 0:1], ALU.mult, ALU.mult, [t_mv, t_rs1], [t_rs1])
                    ACT(S, X1, h1[:], AF.Identity, [t_h1, t_rs1], kx, bias=rs1[:, 1:2], scale=rs1[:, 0:1])
                    TT(S, "dve", X1, X1, ln1g[:], ALU.mult, kx + [t_ln1g], kx)
                    TT(S, "dve", X1, X1, ln1b[:], ALU.add, kx + [t_ln1b], kx)
                    if DBG == "x1":
                        S.dma("sp", (lambda a, b: (lambda e: e.dma_start(out=a, in_=b)))(out_d[tok0:tok0 + 128, :], X1),
                              reads=kx, writes=[], chan=kx[0])
                        continue
                    S.dma("sp", (lambda a, b: (lambda e: e.dma_start(out=a, in_=b)))(x1a_d[tok0:tok0 + 128, :], X1),
                          reads=kx, writes=[], chan=kx[0])
                    CP(S, "act", x1b[:], X1, kx, [t_x1b, t_Pm])
                    for g4 in range(2):
                        for k4 in range(4):
                            kc = g4 * 4 + k4
                            TR(S, P37[:, k4 * 128:(k4 + 1) * 128], X1[:, kc * 128:(kc + 1) * 128], ident_f[:],
                               kx + [t_idf], [tP37])
                        CP(S, "dve", x1T[:, g4 * 4:(g4 + 1) * 4, :], P37[:].rearrange("p (k c) -> p k c", c=128), [tP37], t_mscr)
                    for kc in range(8):
                        MM(S, P45[:, 512:548], x1T[:, kc, :], wr[:, kc, :], kc == 0, kc == 7, t_mscr + [t_wr], [tP45[1]])
                    TT(S, "dve", lg36[:], P45[:, 512:548], rb[:], ALU.add, [tP45[1], t_rb], [t_lg36])
                    S.op("dve", lambda e: e.reduce_max(out=rt[:, 0:1], in_=lg36[:, 0:4], axis=AX.X), [t_lg36], [t_rt])
                    TS(S, "dve", gmask[:], lg36[:, 0:4], rt[:, 0:1], None, ALU.is_equal, None, [t_lg36, t_rt], [t_gmask])
                    TS(S, "dve", rt[:, 1:2], rt[:, 0:1], -1.0, None, ALU.mult, None, [t_rt], [t_rt])
                    ACT(S, gex[:], lg36[:, 0:4], AF.Exp, [t_lg36, t_rt], [t_gex, t_rt], bias=rt[:, 1:2], accum=rt[:, 2:3])
                    S.op("dve", lambda e: e.reciprocal(out=rt[:, 3:4], in_=rt[:, 2:3]), [t_rt], [t_rt])
                    TS(S, "dve", gmask[:], gmask[:], BIG, -BIG, ALU.mult, ALU.add, [t_gmask], [t_gmask])
                    for g in range(4):
                        TS(S, "dve", elm[:, g * 8:(g + 1) * 8], lg36[:, 4 + g * 8:4 + (g + 1) * 8], gmask[:, g:g + 1], None,
                           ALU.add, None, [t_lg36, t_gmask], [t_elm])
                    S.op("dve", lambda e: e.max(out=m8[:], in_=elm[:]), [t_elm], [t_m8])
                    TS(S, "dve", mk1[:], elm[:], m8[:, 0:1], None, ALU.is_equal, None, [t_elm, t_m8], [t_mk1])
                    TS(S, "dve", mk2[:], elm[:], m8[:, 1:2], None, ALU.is_equal, None, [t_elm, t_m8], [t_mk2])
                    TT(S, "dve", rt[:, 4:5], m8[:, 1:2], m8[:, 0:1], ALU.subtract, [t_m8, t_rt], [t_rt])
                    ACT(S, rt[:, 5:6], rt[:, 4:5], AF.Exp, [t_rt], [t_rt])
                    TS(S, "dve", rt[:, 6:7], rt[:, 5:6], 1.0, None, ALU.add, None, [t_rt], [t_rt])
                    S.op("dve", lambda e: e.reciprocal(out=rt[:, 6:7], in_=rt[:, 6:7]), [t_rt], [t_rt])
                    TT(S, "dve", rt[:, 7:8], rt[:, 5:6], rt[:, 6:7], ALU.mult, [t_rt], [t_rt])
                    TT(S, "dve", mkb[:], mk1[:], mk2[:], ALU.add, [t_mk1, t_mk2], [t_mkb])
                    MM(S, P45[:, 576:608], utri[:], mkb[:], True, True, [t_utri, t_mkb], [tP45[1]])
                    MM(S, P45[:, 640:672], ones_b[:], mkb[:], True, True, [t_ones, t_mkb], [tP45[1]])
                    TT(S, "dve", posf[:], P45[:, 576:608], tot[:], ALU.add, [tP45[1], t_tot], [t_posf])
                    TT(S, "dve", tot[:], P45[:, 640:672], tot[:], ALU.add, [tP45[1], t_tot], [t_tot])
                    TS(S, "dve", vld[:], posf[:], float(CAP), None, ALU.is_lt, None, [t_posf], [t_vld])
                    TT(S, "dve", posf[:], posf[:], sbase[:], ALU.add, [t_posf, t_sbase], [t_posf])
                    TS(S, "dve", posf[:], posf[:], -float(NSLOT), None, ALU.add, None, [t_posf], [t_posf])
                    TT(S, "dve", posf[:], posf[:], vld[:], ALU.mult, [t_posf, t_vld], [t_posf])
                    TS(S, "dve", posf[:], posf[:], float(NSLOT), None, ALU.add, None, [t_posf], [t_posf])
                    for k, mk, tmk in ((0, mk1, t_mk1), (1, mk2, t_mk2)):
                        TT(S, "dve", scr32[:], mk[:], posf[:], ALU.mult, [tmk, t_posf], [t_scr32])
                        S.op("dve", (lambda a: (lambda e: e.reduce_sum(out=a, in_=scr32[:], axis=AX.X)))(slf[:, k:k + 1]),
                             [t_scr32], [t_slf])
                        TT(S, "dve", scr32[:], mk[:], vld[:], ALU.mult, [tmk, t_vld], [t_scr32])
                        S.op("dve", (lambda a: (lambda e: e.reduce_sum(out=a, in_=scr32[:], axis=AX.X)))(rt[:, 8 + k:9 + k]),
                             [t_scr32], [t_rt])
                    CP(S, "dve", slot_i[:, tile_i, :], slf[:], [t_slf], [t_slot[tile_i]])
                    TT(S, "dve", rt[:, 10:12], rt[:, 6:8], rt[:, 8:10], ALU.mult, [t_rt], [t_rt])
                    TS(S, "dve", slot_w[:, tile_i, :], rt[:, 10:12], rt[:, 3:4], None, ALU.mult, None, [t_rt], [t_slot[tile_i]])
                    for k in range(2):
                        S.dma("pool", (lambda a, b: (lambda e: e.indirect_dma_start(
                            out=xe_d, out_offset=bass.IndirectOffsetOnAxis(ap=a, axis=0), in_=b, in_offset=None,
                            bounds_check=_bc_reg(e, 1), oob_is_err=False)))(slot_i[:, tile_i, k:k + 1], x1b[:]),
                            reads=[t_x1b, t_slot[tile_i], t_xez], writes=[], chan=t_x1b)
                S.pending = S.defer
                S.defer = None
            if DBG in ("a", "b", "c", "d", "e"):
                S.dma("sp", lambda e: e.dma_start(out=out_d[0:128, :], in_=xs[:, 0, 0, :]), reads=[t_xs[0][0]], writes=[], chan=t_xs[0][0])
            S.barrier()
            S.emit_all()
        S.stack = st
        if DBG is not None:
            return nc

        with contextlib.ExitStack() as st2:
            S.stack = st2
            NST = CAP // 128
            wg = S.sb("wg", [128, 2, 8, DE], BF16); t_wg = [S.tok(), S.tok()]
            wu = S.sb("wu", [128, 2, 8, DE], BF16); t_wu = [S.tok(), S.tok()]
            wd = S.sb("wd", [128, 2, 4, D], BF16); t_wd = [S.tok(), S.tok()]
            xet = S.sb("xet", [128, 2, NST, D], BF16); t_xet = [S.tok(), S.tok()]
            xeT = S.sb("xeT", [128, 2, 8, CAP], BF16); t_xeT = [[S.tok() for _ in range(NST)] for _ in range(2)]
            hT = S.sb("hT", [128, 2, 4, CAP], BF16); t_hT = [[[S.tok() for _ in range(2)] for _ in range(4)] for _ in range(2)]
            sgt = S.sb("sgt", [128, 2, 384], F32); t_sgt = [S.tok(), S.tok()]
            yo = S.sb("yo", [128, 2, D], BF16); t_yo = [S.tok(), S.tok()]

            def load_w(e):
                p = e % 2
                LOAD(S, "pool", wg[:, p], wg_d[e].rearrange("(kc p) c -> p kc c", p=128), t_wg[p])
                LOAD(S, "pool", wu[:, p], wu_d[e].rearrange("(kc p) c -> p kc c", p=128), t_wu[p])
                LOAD(S, "pool", wd[:, p], wd_d[e].rearrange("(kc p) c -> p kc c", p=128), t_wd[p])

            def load_xe(e):
                p = e % 2
                LOAD(S, "sp", xet[:, p], xe_d[e * CAP:(e + 1) * CAP, :].rearrange("(t p) d -> p t d", p=128), t_xet[p])

            def transpose_tile(e, s):
                p = e % 2
                for kc in range(8):
                    TR(S, P2[:, kc * 128:(kc + 1) * 128], xet[:, p, s, kc * 128:(kc + 1) * 128], ident_b[:],
                       [t_xet[p], t_idb], [tP2])
                CP(S, "dve", xeT[:, p, :, s * 128:(s + 1) * 128], P2[:].rearrange("p (k c) -> p k c", c=128),
                   [tP2], [t_xeT[p][s]])

            load_w(0)
            load_xe(0)
            for s in range(NST):
                transpose_tile(0, s)
            for e in range(NE):
                p = e % 2
                if e + 1 < NE:
                    load_w(e + 1)
                    load_xe(e + 1)
                for dc in range(4):
                    dcs = slice(dc * 128, (dc + 1) * 128)
                    for hf in range(2):
                        cs = slice(hf * 384, (hf + 1) * 384)
                        rd = [t_xeT[p][hf * 3 + i] for i in range(3)]
                        u = (dc * 2 + hf) % 2
                        pg = (PA[:, 0:384], PA[:, 512:896])[u]
                        pu = (P1, P6)[u][:, 0:384]
                        tpg = ([tPA[0]], [tPA[1]])[u]
                        tpu = ([tP1], [tP6])[u]
                        for kc in range(8):
                            MM(S, pg, wg[:, p, kc, dcs], xeT[:, p, kc, cs], kc == 0, kc == 7, [t_wg[p]] + rd, tpg)
                        for kc in range(8):
                            MM(S, pu, wu[:, p, kc, dcs], xeT[:, p, kc, cs], kc == 0, kc == 7, [t_wu[p]] + rd, tpu)
                        ACT(S, sgt[:, u, :], pg, AF.Silu, tpg, [t_sgt[u]])
                        TT(S, "dve", hT[:, p, dc, cs], pu, sgt[:, u, :], ALU.mult, list(tpu) + [t_sgt[u]], [t_hT[p][dc][hf]])
                for s in range(NST):
                    q = s % 2
                    hf = s // 3
                    for half in range(2):
                        for dc in range(4):
                            MM(S, P45[:, half * 512:(half + 1) * 512], hT[:, p, dc, s * 128:(s + 1) * 128],
                               wd[:, p, dc, half * 512:(half + 1) * 512], dc == 0, dc == 3,
                               [t_hT[p][dc][hf], t_wd[p]], [tP45[half]])
                    if e + 1 < NE:
                        transpose_tile(e + 1, s)
                    CP(S, "act", yo[:, q, 0:512], P45[:, 0:512], [tP45[0]], [t_yo[q]])
                    CP(S, "dve", yo[:, q, 512:1024], P45[:, 512:1024], [tP45[1]], [t_yo[q]])
                    r0 = e * CAP + s * 128
                    S.dma("sp", (lambda a, b: (lambda e_: e_.dma_start(out=a, in_=b)))(yb_d[r0:r0 + 128, :], yo[:, q, :]),
                          reads=[t_yo[q]], writes=[], chan=t_yo[q])
            S.barrier()
            S.emit_all()
        S.stack = st

        with contextlib.ExitStack() as st3:
            S.stack = st3
            ln2g = S.sb("ln2g", [128, D], F32); t_ln2g = S.tok()
            ln2b = S.sb("ln2b", [128, D], F32); t_ln2b = S.tok()
            LOAD(S, "sp", ln2g[:], ln2g_d, t_ln2g)
            LOAD(S, "sp", ln2b[:], ln2b_d, t_ln2b)
            NG = 4
            y0 = S.sb("y0", [128, NG, D], BF16); t_y0 = [S.tok() for _ in range(NG)]
            y1 = S.sb("y1", [128, NG, D], BF16); t_y1 = [S.tok() for _ in range(NG)]
            xa = S.sb("xa", [128, NG, D], F32); t_xa = [S.tok() for _ in range(NG)]
            h2 = S.sb("h2", [128, 2, D], F32); t_h2 = [S.tok(), S.tok()]
            hn = S.sb("hn", [128, 2, D], F32); t_hn = [S.tok(), S.tok()]
            ho = S.sb("ho", [128, 2, D], F32); t_ho = [S.tok(), S.tok()]
            st2_ = S.sb("st2", [128, 2, 6], F32); t_st2 = S.tok()
            mv2 = S.sb("mv2", [128, 2], F32); t_mv2 = S.tok()
            rs2 = S.sb("rs2", [128, 2], F32); t_rs2 = S.tok()
            for q in range(NG):
                S.op("dve", (lambda a: (lambda e: e.memset(a, 0.0)))(y0[:, q, :]), [], [t_y0[q]])
                S.op("dve", (lambda a: (lambda e: e.memset(a, 0.0)))(y1[:, q, :]), [], [t_y1[q]])

            def gather(ti):
                q = ti % NG
                for k, (yy, tyy) in enumerate(((y0, t_y0), (y1, t_y1))):
                    S.dma("pool", (lambda a, b: (lambda e: e.indirect_dma_start(
                        out=a, out_offset=None, in_=yb_d, in_offset=bass.IndirectOffsetOnAxis(ap=b, axis=0),
                        bounds_check=_bc_reg(e, 3), oob_is_err=False)))(yy[:, q, :], slot_i[:, ti, k:k + 1]),
                        reads=[t_slot[ti]], writes=[tyy[q]], chan=tyy[q])
                LOAD(S, "sp", xa[:, q, :], x1a_d[ti * 128:(ti + 1) * 128, :], t_xa[q])

            for ti in range(min(NG - 1, NTILE)):
                gather(ti)
            for ti in range(NTILE):
                g = ti % NG
                q = ti % 2
                if ti + NG - 1 < NTILE:
                    gather(ti + NG - 1)
                ACT(S, xa[:, g, :], xa[:, g, :], AF.Copy, [t_xa[g]], [t_xa[g]], scale=ALPHA)
                STT(S, hn[:, q, :], y0[:, g, :], slot_w[:, ti, 0:1], xa[:, g, :], ALU.mult, ALU.add,
                    [t_y0[g], t_slot[ti], t_xa[g]], [t_hn[q]])
                STT(S, h2[:, q, :], y1[:, g, :], slot_w[:, ti, 1:2], hn[:, q, :], ALU.mult, ALU.add,
                    [t_y1[g], t_slot[ti], t_hn[q]], [t_h2[q]])
                for half in range(2):
                    S.op("dve", (lambda a, b: (lambda e: e.bn_stats(out=a, in_=b)))(
                        st2_[:, half, :], h2[:, q, half * 512:(half + 1) * 512]), [t_h2[q]], [t_st2])
                S.op("dve", lambda e: e.bn_aggr(out=mv2[:], in_=st2_[:].rearrange("p a b -> p (a b)")), [t_st2], [t_mv2])
                ACT(S, rs2[:, 0:1], mv2[:, 1:2], AF.Sqrt, [t_mv2], [t_rs2], bias=LN_EPS)
                S.op("dve", lambda e: e.reciprocal(out=rs2[:, 0:1], in_=rs2[:, 0:1]), [t_rs2], [t_rs2])
                STT(S, rs2[:, 1:2], mv2[:, 0:1], -1.0, rs2[:, 0:1], ALU.mult, ALU.mult, [t_mv2, t_rs2], [t_rs2])
                ACT(S, hn[:, q, :], h2[:, q, :], AF.Identity, [t_h2[q], t_rs2], [t_hn[q]], bias=rs2[:, 1:2], scale=rs2[:, 0:1])
                TT(S, "dve", hn[:, q, :], hn[:, q, :], ln2g[:], ALU.mult, [t_hn[q], t_ln2g], [t_hn[q]])
                TT(S, "dve", ho[:, q, :], hn[:, q, :], ln2b[:], ALU.add, [t_hn[q], t_ln2b], [t_ho[q]])
                S.dma("sp", (lambda a, b: (lambda e: e.dma_start(out=a, in_=b)))(out_d[ti * 128:(ti + 1) * 128, :], ho[:, q, :]),
                      reads=[t_ho[q]], writes=[], chan=t_ho[q])
            S.barrier()
            S.emit_all()
        S.stack = st
    return nc


def _consts():
    ident = np.eye(128, dtype=np.float32)
    s = np.arange(128)[:, None]
    t = np.arange(128)[None, :]
    bdm = ((s <= t) & ((s // 64) == (t // 64))).astype(np.float32)
    rmask = np.ones((128, TB), np.float32)
    rmask[:, ::64] = 0.0
    slopes = np.exp2(-8.0 * (np.arange(8, dtype=np.float32) + 1.0) / 8.0).astype(np.float32)
    ab = np.zeros((128, 8, 2, 128), np.float32)
    for h in range(8):
        dist_prev = (t + 128 - s).astype(np.float32)
        ab[:, h, 0, :] = np.where(dist_prev < 128, -slopes[h] * dist_prev, -BIG)
        dist_cur = (t - s).astype(np.float32)
        ab[:, h, 1, :] = np.where(dist_cur >= 0, -slopes[h] * dist_cur, -BIG)
    utri = (s < t).astype(np.float32)
    sbase = np.broadcast_to((np.arange(NE, dtype=np.float32) * CAP)[None, :], (128, NE)).copy()
    import ml_dtypes
    return dict(ident=ident, bdmask=bdm, rmask=rmask,
                abias=np.ascontiguousarray(ab.reshape(128, -1).astype(ml_dtypes.bfloat16)), utri=utri, sbase=sbase)


def _prep_shared(inputs):
    f = lambda a: np.ascontiguousarray(np.asarray(a, dtype=np.float32))
    w_in = f(inputs["w_in"])[0]
    aq0 = 2048
    perm = np.arange(PROJ_W)
    newq = []
    for j in range(4):
        for kvh in range(2):
            hd = kvh * 4 + j
            newq.extend(range(aq0 + hd * 64, aq0 + (hd + 1) * 64))
    perm[aq0:aq0 + 512] = np.array(newq)
    w_in_p = np.ascontiguousarray(w_in[:, perm])
    w_b = f(inputs["w_branch_b"])[0]
    w_b_p = np.ascontiguousarray(w_b[np.array(newq) - aq0, :])
    lbl = f(inputs["lb_logits"])
    lbt = np.concatenate([lbl[0].reshape(4, 128).T, lbl[1].reshape(4, 128).T], axis=1)
    sinks = f(inputs["sinks"])[0]
    sinkr = np.zeros((1, 512), np.float32)
    for j in range(4):
        sinkr[0, j * 128:j * 128 + 64] = sinks[j]
        sinkr[0, j * 128 + 64:(j + 1) * 128] = sinks[4 + j]
    rep = lambda v: np.ascontiguousarray(np.broadcast_to(f(v).reshape(1, -1), (128, f(v).size)))
    wr = np.concatenate([f(inputs["router_group_w"])[0], f(inputs["router_expert_w"])[0]], axis=1)
    rbv = np.concatenate([f(inputs["router_group_b"])[0], f(inputs["router_expert_b"])[0]], axis=0)
    d = dict(
        w_in=w_in_p, lbt=np.ascontiguousarray(lbt), nw=f(inputs["hg_norm_w"])[0].reshape(128, 1).copy(),
        sinkr=sinkr, w_a=f(inputs["w_branch_a"])[0], w_b=w_b_p, w_o=f(inputs["w_out"])[0],
        ln1g=rep(inputs["ln1_g"][0]), ln1b=rep(inputs["ln1_b"][0]), ln2g=rep(inputs["ln2_g"][0]), ln2b=rep(inputs["ln2_b"][0]),
        wr=np.ascontiguousarray(wr), rb=rep(rbv),
        wg=f(inputs["w_exp_gate"])[0], wu=f(inputs["w_exp_up"])[0], wd=f(inputs["w_exp_down"])[0],
    )
    d.update(_consts())
    return d


def kernel(**inputs):
    x = np.asarray(inputs["x"], dtype=np.float32)
    B, SQ, _ = x.shape
    per = B // N_CORES
    shared = _prep_shared(inputs)
    nc = build(NSEQ=per, SEQ=SQ)
    in_maps = []
    for c in range(N_CORES):
        m = dict(shared)
        m["x"] = np.ascontiguousarray(x[c * per:(c + 1) * per].reshape(per * SQ, D))
        in_maps.append(m)
    res = run_bass_kernel_spmd(nc, in_maps, core_ids=list(range(N_CORES)))
    outs = [np.asarray(r["out"], dtype=np.float32).reshape(per, SQ, D) for r in res.results]
    return np.concatenate(outs, axis=0)
```

```python
import contextlib
import numpy as np
import concourse.bass as bass
import concourse.mybir as mybir
from concourse.bass_utils import run_bass_kernel_spmd

F32 = mybir.dt.float32
BF16 = mybir.dt.bfloat16
I32 = mybir.dt.int32
AF = mybir.ActivationFunctionType
ALU = mybir.AluOpType
AX = mybir.AxisListType

N_CORES = 8
D = 1024
PROJ_W = 4864
NE = 32
DE = 512
CAP = 768
NSLOT = NE * CAP
ALPHA = 2.0 ** 0.25
LN_EPS = 1e-5
RMS_EPS = 1e-6
TB = 256
BIG = 1.0e30


class Tok:
    __slots__ = ("name", "w", "r", "dsem", "dcount")

    def __init__(self, name):
        self.name = name
        self.w = None
        self.r = {}
        self.dsem = None
        self.dcount = 0


class Sched:
    ENG = ("pe", "act", "dve", "pool", "sp")

    def __init__(self, nc, stack, n_dma_sems=90):
        self.nc = nc
        self.stack = stack
        self.sem_stack = stack
        self.prog = {e: [] for e in self.ENG}
        self.cnt = {e: 0 for e in self.ENG}
        self.seen = {e: {} for e in self.ENG}
        self.esem = {e: stack.enter_context(nc.semaphore("es_" + e)) for e in self.ENG}
        self.dsems = []
        self.n_dma_sems = n_dma_sems
        self.dma_toks = []
        self.nbuf = 0
        self.defer = None
        self.pending = []
        self.in_pump = False
        self.pump_ctr = 0

    def sb(self, name, shape, dtype):
        return self.stack.enter_context(self.nc.sbuf_tensor("s_" + name, list(shape), dtype))

    def ps(self, name, shape, dtype):
        return self.stack.enter_context(self.nc.psum_tensor("p_" + name, list(shape), dtype))

    def tok(self, name=None):
        self.nbuf += 1
        return Tok(name or "t%d" % self.nbuf)

    def _sem_of(self, key):
        if isinstance(key, str):
            return self.esem[key]
        return self.dsems[key[1]]

    def _deps(self, engine, reads, writes):
        deps = {}

        def add(k, v):
            if deps.get(k, 0) < v:
                deps[k] = v
        for t in reads:
            if t.w is not None:
                add(*t.w)
        for t in writes:
            if t.w is not None:
                add(*t.w)
            for k, v in t.r.items():
                add(k, v)
        out = []
        seen = self.seen[engine]
        for k, v in deps.items():
            if k == engine and engine == "pe":
                continue
            if seen.get(k, 0) >= v:
                continue
            seen[k] = v
            out.append((self._sem_of(k), v))
        return out

    def _pump(self, n=1):
        if self.in_pump or not self.pending:
            return
        self.in_pump = True
        for _ in range(n):
            if not self.pending:
                break
            kind, a = self.pending.pop(0)
            (self.op if kind == "op" else self.dma)(*a)
        self.in_pump = False

    def drain(self):
        self._pump(len(self.pending) + 1)

    def op(self, engine, fn, reads=(), writes=()):
        if self.defer is not None:
            self.defer.append(("op", (engine, fn, list(reads), list(writes))))
            return
        self._op(engine, fn, reads, writes)
        self.pump_ctr += 1
        if self.pump_ctr % 3 != 0:
            self._pump()

    def dma(self, queue, fn, reads=(), writes=(), chan=None):
        if self.defer is not None:
            self.defer.append(("dma", (queue, fn, list(reads), list(writes), chan)))
            return
        self._dma(queue, fn, reads, writes, chan)
        self._pump()

    def _op(self, engine, fn, reads=(), writes=()):
        waits = self._deps(engine, reads, writes)
        idx = self.cnt[engine] + 1
        self.cnt[engine] = idx
        sem = self.esem[engine]

        def emit(eng, waits=waits, fn=fn, sem=sem):
            for s, v in waits:
                eng.wait_ge(s, v)
            fn(eng).then_inc(sem, 1)
        self.prog[engine].append(emit)
        for t in reads:
            if t.r.get(engine, 0) < idx:
                t.r[engine] = idx
        for t in writes:
            t.w = (engine, idx)
            t.r = {}
        return idx

    def _dma(self, queue, fn, reads=(), writes=(), chan=None):
        waits = self._deps(queue, reads, writes)
        if chan.dsem is None:
            assert len(self.dsems) < self.n_dma_sems, "out of dma sems"
            chan.dsem = len(self.dsems)
            self.dsems.append(self.sem_stack.enter_context(self.nc.semaphore("ds_%d" % chan.dsem)))
            self.dma_toks.append(chan)
        chan.dcount += 1
        key = ("d", chan.dsem)
        val = 16 * chan.dcount
        sem = self.dsems[chan.dsem]

        def emit(eng, waits=waits, fn=fn, sem=sem):
            for s, v in waits:
                eng.wait_ge(s, v)
            fn(eng).then_inc(sem, 16)
        self.prog[queue].append(emit)
        for t in reads:
            if t.r.get(key, 0) < val:
                t.r[key] = val
        for t in writes:
            t.w = (key, val)
            t.r = {}

    def barrier(self):
        self.drain()
        targets = [(e, self.cnt[e]) for e in self.ENG if self.cnt[e] > 0]
        dtargets = [(("d", t.dsem), 16 * t.dcount) for t in self.dma_toks]
        for eng in self.ENG:
            waits = []
            seen = self.seen[eng]
            for k, v in targets + dtargets:
                if k == eng:
                    continue
                if seen.get(k, 0) >= v:
                    continue
                seen[k] = v
                waits.append((self._sem_of(k), v))

            def emit(e, waits=waits):
                for s, v in waits:
                    e.wait_ge(s, v)
            self.prog[eng].append(emit)

    def emit_all(self):
        nc = self.nc
        with nc.Block() as block:
            @block.tensor
            def _(e):
                for f in self.prog["pe"]:
                    f(e)

            @block.scalar
            def _(e):
                for f in self.prog["act"]:
                    f(e)

            @block.vector
            def _(e):
                for f in self.prog["dve"]:
                    f(e)

            @block.gpsimd
            def _(e):
                for f in self.prog["pool"]:
                    f(e)

            @block.sync
            def _(e):
                for f in self.prog["sp"]:
                    f(e)
        self.prog = {e: [] for e in self.ENG}


def MM(S, out, lhsT, rhs, start, stop, reads, writes):
    S.op("pe", lambda e: e.matmul(out, lhsT=lhsT, rhs=rhs, start=start, stop=stop), reads, writes)


def TR(S, out, in_, ident, reads, writes):
    S.op("pe", lambda e: e.transpose(out=out, in_=in_, identity=ident), reads, writes)


def ACT(S, out, in_, func, reads, writes, bias=None, scale=None, accum=None):
    kw = {}
    if bias is not None:
        kw["bias"] = bias
    if scale is not None:
        kw["scale"] = scale
    if accum is not None:
        kw["accum_out"] = accum
    S.op("act", lambda e: e.activation(out=out, in_=in_, func=func, **kw), reads, writes)


def TS(S, eng, out, in0, s1, s2, op0, op1, reads, writes):
    if op1 is None:
        S.op(eng, lambda e: e.tensor_scalar(out=out, in0=in0, scalar1=s1, scalar2=None, op0=op0), reads, writes)
    else:
        S.op(eng, lambda e: e.tensor_scalar(out=out, in0=in0, scalar1=s1, scalar2=s2, op0=op0, op1=op1), reads, writes)


def TT(S, eng, out, in0, in1, op, reads, writes):
    S.op(eng, lambda e: e.tensor_tensor(out=out, in0=in0, in1=in1, op=op), reads, writes)


def STT(S, out, in0, scalar, in1, op0, op1, reads, writes):
    S.op("dve", lambda e: e.scalar_tensor_tensor(out=out, in0=in0, scalar=scalar, in1=in1, op0=op0, op1=op1), reads, writes)


def CP(S, eng, out, in_, reads, writes):
    if eng == "act":
        S.op("act", lambda e: e.copy(out=out, in_=in_), reads, writes)
    else:
        S.op(eng, lambda e: e.tensor_copy(out=out, in_=in_), reads, writes)


def LOAD(S, queue, out, in_, tok, extra_reads=()):
    S.dma(queue, lambda e: e.dma_start(out=out, in_=in_), reads=list(extra_reads), writes=[tok], chan=tok)


_BC = {}


def _bc_reg(e, phase):
    if _BC.get("nc") is not e:
        _BC.clear()
        _BC["nc"] = e
        _BC["reg"] = e.alloc_register("bc")
    if _BC.get("phase") != phase:
        e.reg_mov(_BC["reg"], NSLOT - 1)
        _BC["phase"] = phase
    return _BC["reg"]


def build(NSEQ=4, SEQ=2048, DBG=None):
    NTOK = NSEQ * SEQ
    NSB = SEQ // TB
    NTILE = NTOK // 128
    nc = bass.Bass("TRN2", target_bir_lowering=False)

    def din(name, shape, dtype=F32):
        return nc.dram_tensor(name, list(shape), dtype, kind="ExternalInput").ap()

    x_d = din("x", [NTOK, D])
    win_d = din("w_in", [D, PROJ_W])
    lbt_d = din("lbt", [128, 8])
    nw_d = din("nw", [128, 1])
    wa_d = din("w_a", [512, D])
    wb_d = din("w_b", [512, D])
    wo_d = din("w_o", [D, D])
    ln1g_d = din("ln1g", [128, D])
    ln1b_d = din("ln1b", [128, D])
    ln2g_d = din("ln2g", [128, D])
    ln2b_d = din("ln2b", [128, D])
    wr_d = din("wr", [D, 36])
    rb_d = din("rb", [128, 36])
    wg_d = din("wg", [NE, D, DE])
    wu_d = din("wu", [NE, D, DE])
    wd_d = din("wd", [NE, DE, D])
    ident_d = din("ident", [128, 128])
    bdm_d = din("bdmask", [128, 128])
    rmask_d = din("rmask", [128, TB])
    abias_d = din("abias", [128, 8 * 2 * 128], BF16)
    sinkr_d = din("sinkr", [1, 512])
    utri_d = din("utri", [128, 128])
    sbase_d = din("sbase", [128, NE])
    out_d = nc.dram_tensor("out", [NTOK, D], F32, kind="ExternalOutput").ap()
    x1a_d = nc.dram_tensor("x1a", [NTOK, D], F32, kind="Internal").ap()
    xe_d = nc.dram_tensor("xe", [NSLOT, D], BF16, kind="Internal").ap()
    yb_d = nc.dram_tensor("yb", [NSLOT, D], BF16, kind="Internal").ap()

    with contextlib.ExitStack() as st:
        S = Sched(nc, st)
        PA = S.ps("PA", [128, 1024], F32); tPA = [S.tok(), S.tok()]
        P1 = S.ps("P1", [128, 512], F32); tP1 = S.tok()
        P2 = S.ps("P2", [128, 1024], BF16); tP2 = S.tok()
        P45 = S.ps("P45", [128, 1024], F32); tP45 = [S.tok(), S.tok()]
        P6 = S.ps("P6", [128, 512], F32); tP6 = S.tok()
        P37 = S.ps("P37", [128, 512], F32); tP37 = S.tok()
        ident_f = S.sb("ident_f", [128, 128], F32); t_idf = S.tok()
        ident_b = S.sb("ident_b", [128, 128], BF16); t_idb = S.tok()
        slot_i = S.sb("slot_i", [128, NTILE, 2], I32)
        slot_w = S.sb("slot_w", [128, NTILE, 2], F32)
        t_slot = [S.tok() for _ in range(NTILE)]
        LOAD(S, "sp", ident_f[:], ident_d, t_idf)
        CP(S, "dve", ident_b[:], ident_f[:], [t_idf], [t_idb])

        with contextlib.ExitStack() as st1:
            S.stack = st1
            win = S.sb("win", [128, 8, PROJ_W], BF16); t_win = S.tok()
            wa = S.sb("wa", [128, 4, D], BF16); t_wa = S.tok()
            wb = S.sb("wb", [128, 4, D], BF16); t_wb = S.tok()
            wo = S.sb("wo", [128, 8, D], BF16); t_wo = S.tok()
            win_v = win_d.rearrange("(kc p) c -> p kc c", p=128)
            for c4 in range(4):
                S.dma("pool", (lambda a, b: (lambda e: e.dma_start(out=a, in_=b)))(
                    win[:, :, c4 * 1216:(c4 + 1) * 1216], win_v[:, :, c4 * 1216:(c4 + 1) * 1216]),
                    writes=[t_win], chan=t_win)
            LOAD(S, "pool", wa[:], wa_d.rearrange("(kc p) c -> p kc c", p=128), t_wa)
            LOAD(S, "pool", wb[:], wb_d.rearrange("(kc p) c -> p kc c", p=128), t_wb)
            LOAD(S, "pool", wo[:], wo_d.rearrange("(kc p) c -> p kc c", p=128), t_wo)
            bdm = S.sb("bdm", [128, 128], F32); t_bdm = S.tok()
            rmask = S.sb("rmask", [128, TB], F32); t_rmask = S.tok()
            abias = S.sb("abias", [128, 8 * 2 * 128], BF16); t_abias = S.tok()
            utri_f = S.sb("utri_f", [128, 128], F32); t_utf = S.tok()
            utri = S.sb("utri", [128, 128], BF16); t_utri = S.tok()
            ones_b = S.sb("ones_b", [128, 128], BF16); t_ones = S.tok()
            onesv = S.sb("onesv", [128, 128], BF16); t_onesv = S.tok()
            sbase = S.sb("sbase", [128, NE], F32); t_sbase = S.tok()
            lbt = S.sb("lbt", [128, 8], F32); t_lbt = S.tok()
            lbc = S.sb("lbc", [128, 4], F32); t_lbc = S.tok()
            oml = S.sb("oml", [128, 4], F32); t_oml = S.tok()
            nw = S.sb("nw", [128, 1], F32); t_nw = S.tok()
            ln1g = S.sb("ln1g", [128, D], F32); t_ln1g = S.tok()
            ln1b = S.sb("ln1b", [128, D], F32); t_ln1b = S.tok()
            wr = S.sb("wr", [128, 8, 36], F32); t_wr = S.tok()
            rb = S.sb("rb", [128, 36], F32); t_rb = S.tok()
            tot = S.sb("tot", [128, NE], F32); t_tot = S.tok()
            LOAD(S, "sp", bdm[:], bdm_d, t_bdm)
            LOAD(S, "sp", rmask[:], rmask_d, t_rmask)
            LOAD(S, "sp", abias[:], abias_d, t_abias)
            LOAD(S, "sp", utri_f[:], utri_d, t_utf)
            LOAD(S, "sp", sbase[:], sbase_d, t_sbase)
            LOAD(S, "sp", lbt[:], lbt_d, t_lbt)
            LOAD(S, "sp", nw[:], nw_d, t_nw)
            LOAD(S, "sp", ln1g[:], ln1g_d, t_ln1g)
            LOAD(S, "sp", ln1b[:], ln1b_d, t_ln1b)
            LOAD(S, "sp", wr[:], wr_d.rearrange("(kc p) c -> p kc c", p=128), t_wr)
            LOAD(S, "sp", rb[:], rb_d, t_rb)
            CP(S, "dve", utri[:], utri_f[:], [t_utf], [t_utri])
            S.op("dve", lambda e: e.memset(ones_b[:], 1.0), [], [t_ones])
            S.op("dve", lambda e: e.memset(onesv[:], 1.0 / 128.0), [], [t_onesv])
            S.op("dve", lambda e: e.memset(tot[:], 0.0), [], [t_tot])
            TT(S, "dve", lbc[:], lbt[:, 0:4], lbt[:, 4:8], ALU.subtract, [t_lbt], [t_lbc])
            ACT(S, lbc[:], lbc[:], AF.Sigmoid, [t_lbc], [t_lbc])
            TS(S, "dve", oml[:], lbc[:], -1.0, 1.0, ALU.mult, ALU.add, [t_lbc], [t_oml])
            xs = S.sb("xs", [128, 2, 2, D], F32)
            t_xs = [[S.tok(), S.tok()], [S.tok(), S.tok()]]
            xb = S.sb("xb", [128, D], BF16); t_xb = S.tok()
            xT = S.sb("xT", [128, 8, TB], BF16); t_xT = [S.tok(), S.tok()]
            qs = S.sb("qs", [128, TB], F32); t_qs = S.tok()
            ebt = S.sb("ebt", [128, TB], F32); t_ebt = S.tok()
            ebl = S.sb("ebl", [128, 4, 4], F32); t_ebl = [S.tok() for _ in range(4)]
            tmp1 = S.sb("tmp1", [128, TB], F32); t_tmp1 = S.tok()
            tmp2 = S.sb("tmp2", [128, TB], F32); t_tmp2 = S.tok()
            tmp3 = S.sb("tmp3", [128, TB], F32); t_tmp3 = S.tok()
            Qt = S.sb("Qt", [128, 4, TB], BF16); t_Qt = [S.tok() for _ in range(4)]
            Kt = S.sb("Kt", [128, 4, TB], BF16); t_Kt = [S.tok() for _ in range(4)]
            Kh = S.sb("Kh", [128, 4, TB], BF16); t_Kh = [S.tok() for _ in range(4)]
            gs = S.sb("gs", [128, 4, TB], BF16); t_gs = [S.tok() for _ in range(4)]
            aqT = S.sb("aqT", [128, 4, TB], BF16); t_aq = [S.tok() for _ in range(4)]
            kT = S.sb("kT", [128, 128 + TB], BF16); t_kT = S.tok(); t_kTp = S.tok()
            vt = S.sb("vt", [128, 2, 512], BF16); t_vt = [S.tok(), S.tok()]
            avt = S.sb("avt", [128, 3, 128], BF16); t_avt = [S.tok() for _ in range(3)]
            S_f = S.sb("S_f", [128, 4, 128], F32); t_Sf = [S.tok() for _ in range(4)]
            S_b = S.sb("S_b", [128, 4, 128], BF16); t_Sb = [S.tok() for _ in range(4)]
            sm = S.sb("sm", [128, 4, 128], BF16); t_sm = [S.tok() for _ in range(4)]
            khtok = S.sb("khtok", [128, 4, 128], BF16); t_kht = [S.tok() for _ in range(4)]
            bdm4 = S.sb("bdm4", [128, 512], BF16); t_bdm4 = S.tok()
            for h in range(4):
                CP(S, "dve", bdm4[:, h * 128:(h + 1) * 128], bdm[:], [t_bdm], [t_bdm4])
            o2 = S.sb("o2", [128, 2, TB], BF16); t_o2 = [S.tok(), S.tok()]
            ofin = S.sb("ofin", [128, 4, TB], BF16); t_ofin = [S.tok() for _ in range(4)]
            lg = S.sb("lg", [128, 1024], F32); t_lg = S.tok()
            Pm = S.sb("Pm", [128, 1024], BF16); t_Pm = S.tok()
            rr = S.sb("rr", [128, 512], F32); t_rr = S.tok()
            esr = S.sb("esr", [1, 512], F32); t_esr = S.tok()
            ones_r = S.sb("ones_r", [1, 256], F32); t_onesr = S.tok()
            LOAD(S, "sp", esr[:], sinkr_d, t_esr)
            ACT(S, esr[:], esr[:], AF.Exp, [t_esr], [t_esr])
            S.op("dve", lambda e: e.memset(ones_r[:], 1.0), [], [t_onesr])
            attT = S.sb("attT", [128, 4, TB], BF16); t_att = [S.tok() for _ in range(4)]
            mT = S.sb("mT", [128, 8, TB], BF16); t_mT = [S.tok() for _ in range(8)]
            mscr = S.sb("mscr", [128, 4, TB], F32)
            sga = mscr[:, 0, :]; t_sga = S.tok()
            sgb = mscr[:, 1, :]; t_sgb = S.tok()
            mt1 = mscr[:, 2, :]; t_mt1 = S.tok()
            mt2 = mscr[:, 3, :]; t_mt2 = S.tok()
            t_mscr = [t_sga, t_sgb, t_mt1, t_mt2]
            h1 = S.sb("h1", [128, D], F32); t_h1 = S.tok()
            o_f = h1[:, 0:512].rearrange("p (b c) -> p b c", c=TB); t_of = [S.tok(), S.tok()]
            rstd = h1[:, 512:1024].rearrange("p (b c) -> p b c", c=TB); t_rstd = [S.tok(), S.tok()]
            x1 = S.sb("x1", [128, D], F32); t_x1 = S.tok()
            x1_bufs = (x1[:], mT[:].rearrange("p a b -> p (a b)").bitcast(F32))
            x1_toks = ([t_x1], [S.tok()] + t_mT)
            x1al = h1; t_x1al = t_h1
            x1b = Pm; t_x1b = S.tok()
            x1T = mscr[:].rearrange("p a (b c) -> p (a b) c", c=128)
            stats = S.sb("stats", [128, 2, 6], F32); t_stats = S.tok()
            mv = S.sb("mv", [128, 2], F32); t_mv = S.tok()
            rs1 = S.sb("rs1", [128, 2], F32); t_rs1 = S.tok()
            lg36 = S.sb("lg36", [128, 36], F32); t_lg36 = S.tok()
            rt = S.sb("rt", [128, 16], F32); t_rt = S.tok()
            gmask = S.sb("gmask", [128, 4], F32); t_gmask = S.tok()
            gex = S.sb("gex", [128, 4], F32); t_gex = S.tok()
            elm = S.sb("elm", [128, 32], F32); t_elm = S.tok()
            m8 = S.sb("m8", [128, 8], F32); t_m8 = S.tok()
            mk1 = S.sb("mk1", [128, 32], F32); t_mk1 = S.tok()
            mk2 = S.sb("mk2", [128, 32], F32); t_mk2 = S.tok()
            mkb = S.sb("mkb", [128, 32], BF16); t_mkb = S.tok()
            posf = S.sb("posf", [128, 32], F32); t_posf = S.tok()
            vld = S.sb("vld", [128, 32], F32); t_vld = S.tok()
            scr32 = S.sb("scr32", [128, 32], F32); t_scr32 = S.tok()
            slf = S.sb("slf", [128, 2], F32); t_slf = S.tok()

            t_xez = S.tok()
            S.op("pool", lambda e: e.memset(Pm[:], 0.0), [], [t_Pm])
            if DBG is None:
                xe_v = xe_d.rearrange("(n p) d -> p n d", p=128)
                nrow = NSLOT // 128
                for r0 in range(0, nrow, 1):
                    S.dma("sp", (lambda a, b: (lambda e: e.dma_start(out=a, in_=b)))(
                        xe_v[:, r0:r0 + 1, :], Pm[:].rearrange("p (n d) -> p n d", d=D)),
                        reads=[t_Pm], writes=[t_xez], chan=t_xez)

            pj_state = [0]
            tokB = [S.tok() for _ in range(5)]
            esets = (
                (tmp1[:], tmp2[:], tmp3[:], qs[:], ebt[:], t_tmp1, t_tmp2, t_tmp3, t_qs, t_ebt, [], []),
                (lg[:, 0:256], lg[:, 256:512], lg[:, 512:768], lg[:, 768:1024], rr[:, 0:256],
                 tokB[0], tokB[1], tokB[2], tokB[3], tokB[4], [t_lg], [t_rr]),
            )
            po_loc = ((P45, 0), (P45, 512), (P6, 0), (PA, 256))
            po_tok = (tP45[0], tP45[1], tP6, tPA[0])

            def po_ap(h, c0, n):
                pt, off = po_loc[h]
                return pt[:, off + c0:off + c0 + n]

            def projF(col0, evac):
                u = pj_state[0] % 2
                pj_state[0] += 1
                pp = PA[:, u * 512:u * 512 + 256]
                for kc in range(8):
                    MM(S, pp, win[:, kc, col0:col0 + 128], xT[:, kc, :], kc == 0, kc == 7,
                       [t_win, t_xT[0], t_xT[1]], [tPA[u]])
                evac(pp, tPA[u])

            def load_x(sbi):
                par = sbi % 2
                g0 = sbi * TB
                for t in range(2):
                    LOAD(S, "sp", xs[:, par, t, :], x_d[g0 + t * 128:g0 + (t + 1) * 128, :], t_xs[par][t])

            load_x(0)
            for sbi in range(NSEQ * NSB):
                par = sbi % 2
                jb = sbi % NSB
                g0 = sbi * TB
                if jb == 0:
                    for h in range(4):
                        S.op("dve", (lambda a: (lambda e: e.memset(a, 0.0)))(S_f[:, h, :]), [], [t_Sf[h]])
                        S.op("pool", (lambda a: (lambda e: e.memset(a, 0.0)))(S_b[:, h, :]), [], [t_Sb[h]])
                for t in range(2):
                    CP(S, "act", xb[:], xs[:, par, t, :], [t_xs[par][t]], [t_xb])
                    for kc in range(8):
                        TR(S, P2[:, kc * 128:(kc + 1) * 128], xb[:, kc * 128:(kc + 1) * 128], ident_b[:],
                           [t_xb, t_idb], [tP2])
                    CP(S, "dve", xT[:, :, t * 128:(t + 1) * 128], P2[:].rearrange("p (k c) -> p k c", c=128),
                       [tP2], [t_xT[t]])
                for t in range(2):
                    u = pj_state[0] % 2
                    pj_state[0] += 1
                    for kc in range(8):
                        MM(S, PA[:, u * 512:(u + 1) * 512], xT[:, kc, t * 128:(t + 1) * 128], win[:, kc, 1024:1536],
                           kc == 0, kc == 7, [t_win, t_xT[t]], [tPA[u]])
                    CP(S, "act", vt[:, t, :], PA[:, u * 512:(u + 1) * 512], [tPA[u]], [t_vt[t]])
                    u = pj_state[0] % 2
                    pj_state[0] += 1
                    for kc in range(8):
                        MM(S, PA[:, u * 512:u * 512 + 128], xT[:, kc, t * 128:(t + 1) * 128], win[:, kc, 2688:2816],
                           kc == 0, kc == 7, [t_win, t_xT[t]], [tPA[u]])
                    CP(S, "dve", avt[:, 1 + t, :], PA[:, u * 512:u * 512 + 128], [tPA[u]], [t_avt[1 + t]])
                for j in range(4):
                    projF(2048 + j * 128, lambda pp, tk, j=j: CP(S, "act", aqT[:, j, :], pp, [tk], [t_aq[j]]))
                projF(2560, lambda pp, tk: CP(S, "dve", kT[:, 128:128 + TB], pp, [tk], [t_kT]))
                if DBG == "a":
                    break
                def el_stage1(h):
                    T1, T2, T3, Q_, E_, k1, k2, k3, kq, ke, xw, xe = esets[h % 2]
                    projF(0 + h * 128, lambda pp, tk: ACT(S, Q_, pp, AF.Silu, [tk], [kq] + xw))
                    projF(1536 + h * 128, lambda pp, tk: ACT(S, gs[:, h, :], pp, AF.Silu, [tk], [t_gs[h]]))
                    projF(512 + h * 128, lambda pp, tk: ACT(S, T1, pp, AF.Sigmoid, [tk], [k1] + xw))
                    TS(S, "dve", T3, T1, oml[:, h:h + 1], lbc[:, h:h + 1], ALU.mult, ALU.add,
                       [k1, t_oml, t_lbc], [k3] + xw)

                def el_stage2(h):
                    T1, T2, T3, Q_, E_, k1, k2, k3, kq, ke, xw, xe = esets[h % 2]
                    ACT(S, T2, T3, AF.Ln, [k3], [k2] + xw)
                    S.op("dve", (lambda o_, d1: (lambda e: e.tensor_tensor_scan(out=o_, data0=rmask[:], data1=d1, initial=0.0,
                                                                               op0=ALU.mult, op1=ALU.add)))(T1, T2),
                         [t_rmask, k2], [k1] + xw)
                    ACT(S, E_, T1, AF.Exp, [k1], [ke] + xe)
                    CP(S, "pool", ebl[:, h, :], E_[:, 63:TB:64], [ke], [t_ebl[h]])
                    ACT(S, T2, T1, AF.Exp, [k1], [k2] + xw, scale=-1.0)
                    TT(S, "dve", Qt[:, h, :], Q_, E_, ALU.mult, [kq, ke], [t_Qt[h]])
                    TS(S, "pool", Q_, T3, -1.0, 1.0, ALU.mult, ALU.add, [k3], [kq] + xw)
                    TT(S, "pool", Kt[:, h, :], Q_, T2, ALU.mult, [kq, k2], [t_Kt[h]])
                    for c in range(4):
                        TS(S, "dve", Kh[:, h, c * 64:(c + 1) * 64], Kt[:, h, c * 64:(c + 1) * 64],
                           ebl[:, h, c:c + 1], None, ALU.mult, None, [t_Kt[h], t_ebl[h]], [t_Kh[h]])

                el_stage1(0)
                el_stage1(1)
                el_stage2(0)
                el_stage1(2)
                el_stage2(1)
                el_stage1(3)
                el_stage2(2)
                el_stage2(3)
                if DBG == "b":
                    break
                S.drain()
                if sbi + 1 < NSEQ * NSB and DBG not in ("a", "b", "c", "d", "e"):
                    load_x(sbi + 1)
                for t in range(2):
                    tc0 = t * 128
                    for h in range(4):
                        MM(S, P37[:, h * 128:(h + 1) * 128], Kt[:, h, tc0:tc0 + 128], Qt[:, h, tc0:tc0 + 128], True, True,
                           [t_Kt[h], t_Qt[h]], [tP37])
                        TR(S, P2[:, h * 128:(h + 1) * 128], Kh[:, h, tc0:tc0 + 128], ident_b[:], [t_Kh[h], t_idb], [tP2])
                    TT(S, "dve", sm[:].rearrange("p h c -> p (h c)"), P37[:, :], bdm4[:], ALU.mult, [tP37, t_bdm4], t_sm)
                    CP(S, "act", khtok[:].rearrange("p h c -> p (h c)"), P2[:, 0:512], [tP2], t_kht)
                    for c in range(2):
                        cc0 = tc0 + c * 64
                        for h in range(4):
                            if c == 0:
                                MM(S, po_ap(h, tc0, 128), vt[:, t, h * 128:(h + 1) * 128], sm[:, h, :],
                                   True, False, [t_vt[t], t_sm[h]], [po_tok[h]])
                            MM(S, po_ap(h, cc0, 64), S_b[:, h, :], Qt[:, h, cc0:cc0 + 64],
                               False, c == 1, [t_Sb[h], t_Qt[h]], [po_tok[h]])
                            MM(S, P1[:, h * 128:(h + 1) * 128], khtok[c * 64:(c + 1) * 64, h, :],
                               vt[c * 64:(c + 1) * 64, t, h * 128:(h + 1) * 128], True, True, [t_kht[h], t_vt[t]], [tP1])
                        for h in range(4):
                            STT(S, S_f[:, h, :], S_f[:, h, :], ebl[:, h, t * 2 + c:t * 2 + c + 1], P1[:, h * 128:(h + 1) * 128],
                                ALU.mult, ALU.add, [t_Sf[h], t_ebl[h], tP1], [t_Sf[h]])
                        CP(S, "act", S_b[:].rearrange("p h c -> p (h c)"), S_f[:].rearrange("p h c -> p (h c)"), t_Sf, t_Sb)
                for h in range(4):
                    bb = h % 2
                    ACT(S, o2[:, bb, :], po_ap(h, 0, TB), AF.Square, [po_tok[h]], [t_o2[bb]])
                    pms = PA[:, 512:512 + 256]
                    MM(S, pms, onesv[:], o2[:, bb, :], True, True, [t_onesv, t_o2[bb]], [tPA[1]])
                    ACT(S, rstd[:, bb, :], pms, AF.Ln, [tPA[1]], [t_rstd[bb], t_h1], bias=RMS_EPS)
                    ACT(S, rstd[:, bb, :], rstd[:, bb, :], AF.Exp, [t_rstd[bb]], [t_rstd[bb]], scale=-0.5)
                    STT(S, o_f[:, bb, :], po_ap(h, 0, TB), nw[:, 0:1], rstd[:, bb, :], ALU.mult, ALU.mult,
                        [po_tok[h], t_nw, t_rstd[bb]], [t_of[bb], t_h1])
                    TT(S, "dve", ofin[:, h, :], o_f[:, bb, :], gs[:, h, :], ALU.mult, [t_of[bb], t_gs[h]], [t_ofin[h]])
                if DBG == "c":
                    break
                items = [(t, jp) for t in range(2) for jp in range(2)]

                def att_scores(t, jp):
                    tc0 = t * 128
                    has_prev = not (jb == 0 and t == 0)
                    pcs = (0, 1) if has_prev else (1,)
                    for jl in range(2):
                        j = jp * 2 + jl
                        for pc in pcs:
                            kc0 = tc0 + pc * 128
                            col = (jl * 2 + pc) * 128
                            for hh in range(2):
                                r0 = hh * 64
                                MM(S, (P1, P37)[hh][:, col:col + 128], kT[r0:r0 + 64, kc0:kc0 + 128],
                                   aqT[r0:r0 + 64, j, tc0:tc0 + 128], True, True,
                                   [t_kT, t_kTp, t_aq[j]], [(tP1, tP37)[hh]])

                att_scores(*items[0])
                for it, (t, jp) in enumerate(items):
                    tc0 = t * 128
                    has_prev = not (jb == 0 and t == 0)
                    pcs = (0, 1) if has_prev else (1,)
                    lg5 = lg[:].rearrange("p (a h c) -> p a h c", h=2, c=128)
                    for hh in range(2):
                        bank = (P1, P37)[hh]
                        tb_ = (tP1, tP37)[hh]
                        ab0 = (hh * 4 + jp * 2) * 256
                        if has_prev:
                            STT(S, lg5[:, :, hh, :], bank[:, :].rearrange("p (a c) -> p a c", c=128), 0.125,
                                abias[:, ab0:ab0 + 512].rearrange("p (a c) -> p a c", c=128),
                                ALU.mult, ALU.add, [tb_, t_abias], [t_lg] + tokB[0:4])
                        else:
                            for jl in range(2):
                                a_i = jl * 2 + 1
                                STT(S, lg5[:, a_i, hh, :], bank[:, a_i * 128:(a_i + 1) * 128], 0.125,
                                    abias[:, ab0 + a_i * 128:ab0 + (a_i + 1) * 128],
                                    ALU.mult, ALU.add, [tb_, t_abias], [t_lg] + tokB[0:4])
                    if has_prev:
                        ACT(S, Pm[:], lg[:], AF.Exp, [t_lg], [t_Pm, t_x1b])
                    else:
                        ACT(S, Pm[:].rearrange("p (j q c) -> p j q c", q=2, c=256)[:, :, 1, :],
                            lg[:].rearrange("p (j q c) -> p j q c", q=2, c=256)[:, :, 1, :], AF.Exp, [t_lg], [t_Pm, t_x1b])
                    if it + 1 < len(items):
                        att_scores(*items[it + 1])
                    for jl in range(2):
                        for k_i, pc in enumerate(pcs):
                            seg = Pm[:, (jl * 2 + pc) * 256:(jl * 2 + pc + 1) * 256]
                            MM(S, P6[:, jl * 256:(jl + 1) * 256], avt[:, t + pc, :], seg, k_i == 0, k_i == len(pcs) - 1,
                               [t_avt[t + pc], t_Pm], [tP6])
                    for jl in range(2):
                        j = jp * 2 + jl
                        for k_i, pc in enumerate(pcs):
                            seg = Pm[:, (jl * 2 + pc) * 256:(jl * 2 + pc + 1) * 256]
                            MM(S, PA[:, jl * 256:(jl + 1) * 256], ones_b[:], seg, k_i == 0, False,
                               [t_ones, t_Pm], [tPA[0]])
                        MM(S, PA[:, jl * 256:(jl + 1) * 256], esr[0:1, j * 128:(j + 1) * 128], ones_r[0:1, :], False, True,
                           [t_esr, t_onesr], [tPA[0]])
                    ACT(S, rr[:], PA[:, 0:512], AF.Ln, [tPA[0]], [t_rr, tokB[4]])
                    ACT(S, rr[:], rr[:], AF.Exp, [t_rr], [t_rr], scale=-1.0)
                    for hh in range(2):
                        r0 = hh * 64
                        TT(S, "dve", attT[r0:r0 + 64, jp * 2:jp * 2 + 2, tc0:tc0 + 128],
                           P6[r0:r0 + 64, :].rearrange("p (j c) -> p j c", c=256)[:, :, hh * 128:(hh + 1) * 128],
                           rr[r0:r0 + 64, :].rearrange("p (j c) -> p j c", c=256)[:, :, hh * 128:(hh + 1) * 128],
                           ALU.mult, [tP6, t_rr], [t_att[jp * 2], t_att[jp * 2 + 1]])
                CP(S, "pool", kT[:, 0:128], kT[:, TB:TB + 128], [t_kT], [t_kTp])
                CP(S, "pool", avt[:, 0, :], avt[:, 2, :], [t_avt[2]], [t_avt[0]])
                if DBG == "d":
                    break
                for fc in range(8):
                    fcs = slice(fc * 128, (fc + 1) * 128)
                    bank = (P6, P1)[fc % 2]
                    tb_ = (tP6, tP1)[fc % 2]
                    for h in range(4):
                        MM(S, bank[:, 0:256], wa[:, h, fcs], ofin[:, h, :], h == 0, h == 3, [t_wa, t_ofin[h]], [tb_])
                    for j in range(4):
                        MM(S, bank[:, 256:512], wb[:, j, fcs], attT[:, j, :], j == 0, j == 3, [t_wb, t_att[j]], [tb_])
                    projF(2816 + fc * 128, lambda pp, tk: ACT(S, sga, pp, AF.Sigmoid, [tk], [t_sga]))
                    projF(3840 + fc * 128, lambda pp, tk: ACT(S, sgb, pp, AF.Sigmoid, [tk], [t_sgb]))
                    TT(S, "dve", mt1, bank[:, 0:256], sga, ALU.mult, [tb_, t_sga], [t_mt1])
                    TT(S, "dve", mt2, bank[:, 256:512], sgb, ALU.mult, [tb_, t_sgb], [t_mt2])
                    TT(S, "pool", mT[:, fc, :], mt1, mt2, ALU.add, [t_mt1, t_mt2], [t_mT[fc]])
                if DBG == "e":
                    break
                op_loc = ((P45[:, 0:512], P45[:, 512:1024], tP45[0], tP45[1]), (P6[:, :], P1[:, :], tP6, tP1))
                for t in range(2):
                    tc0 = t * 128
                    for half in range(2):
                        for fc in range(8):
                            MM(S, op_loc[t][half], mT[:, fc, tc0:tc0 + 128],
                               wo[:, fc, half * 512:(half + 1) * 512], fc == 0, fc == 7,
                               [t_mT[fc], t_wo], [op_loc[t][2 + half]])
                S.defer = []
                for t in range(2):
                    tile_i = sbi * 2 + t
                    tc0 = t * 128
                    tok0 = g0 + tc0
                    X1 = x1_bufs[t]
                    kx = x1_toks[t]
                    for half in range(2):
                        STT(S, h1[:, half * 512:(half + 1) * 512], xs[:, par, t, half * 512:(half + 1) * 512], ALPHA,
                            op_loc[t][half], ALU.mult, ALU.add, [t_xs[par][t], op_loc[t][2 + half]], [t_h1] + t_of + t_rstd)
                    for half in range(2):
                        S.op("dve", (lambda a, b: (lambda e: e.bn_stats(out=a, in_=b)))(
                            stats[:, half, :], h1[:, half * 512:(half + 1) * 512]), [t_h1], [t_stats])
                    S.op("dve", lambda e: e.bn_aggr(out=mv[:], in_=stats[:].rearrange("p a b -> p (a b)")), [t_stats], [t_mv])
                    ACT(S, rs1[:, 0:1], mv[:, 1:2], AF.Ln, [t_mv], [t_rs1], bias=LN_EPS)
                    ACT(S, rs1[:, 0:1], rs1[:, 0:1], AF.Exp, [t_rs1], [t_rs1], scale=-0.5)
                    STT(S, rs1[:, 1:2], mv[:, 0:1], -1.0, rs1[:, 0:1], ALU.mult, ALU.mult, [t_mv, t_rs1], [t_rs1])
                    ACT(S, X1, h1[:], AF.Identity, [t_h1, t_rs1], kx, bias=rs1[:, 1:2], scale=rs1[:, 0:1])
                    TT(S, "dve", X1, X1, ln1g[:], ALU.mult, kx + [t_ln1g], kx)
                    TT(S, "dve", X1, X1, ln1b[:], ALU.add, kx + [t_ln1b], kx)
                    if DBG == "x1":
                        S.dma("sp", (lambda a, b: (lambda e: e.dma_start(out=a, in_=b)))(out_d[tok0:tok0 + 128, :], X1),
                              reads=kx, writes=[], chan=kx[0])
                        continue
                    S.dma("sp", (lambda a, b: (lambda e: e.dma_start(out=a, in_=b)))(x1a_d[tok0:tok0 + 128, :], X1),
                          reads=kx, writes=[], chan=kx[0])
                    CP(S, "act", x1b[:], X1, kx, [t_x1b, t_Pm])
                    for g4 in range(2):
                        for k4 in range(4):
                            kc = g4 * 4 + k4
                            TR(S, P37[:, k4 * 128:(k4 + 1) * 128], X1[:, kc * 128:(kc + 1) * 128], ident_f[:],
                               kx + [t_idf], [tP37])
                        CP(S, "dve", x1T[:, g4 * 4:(g4 + 1) * 4, :], P37[:].rearrange("p (k c) -> p k c", c=128), [tP37], t_mscr)
                    for kc in range(8):
                        MM(S, P45[:, 512:548], x1T[:, kc, :], wr[:, kc, :], kc == 0, kc == 7, t_mscr + [t_wr], [tP45[1]])
                    TT(S, "dve", lg36[:], P45[:, 512:548], rb[:], ALU.add, [tP45[1], t_rb], [t_lg36])
                    S.op("dve", lambda e: e.reduce_max(out=rt[:, 0:1], in_=lg36[:, 0:4], axis=AX.X), [t_lg36], [t_rt])
                    TS(S, "dve", gmask[:], lg36[:, 0:4], rt[:, 0:1], None, ALU.is_equal, None, [t_lg36, t_rt], [t_gmask])
                    TS(S, "dve", rt[:, 1:2], rt[:, 0:1], -1.0, None, ALU.mult, None, [t_rt], [t_rt])
                    ACT(S, gex[:], lg36[:, 0:4], AF.Exp, [t_lg36, t_rt], [t_gex, t_rt], bias=rt[:, 1:2], accum=rt[:, 2:3])
                    S.op("dve", lambda e: e.reciprocal(out=rt[:, 3:4], in_=rt[:, 2:3]), [t_rt], [t_rt])
                    TS(S, "dve", gmask[:], gmask[:], BIG, -BIG, ALU.mult, ALU.add, [t_gmask], [t_gmask])
                    for g in range(4):
                        TS(S, "dve", elm[:, g * 8:(g + 1) * 8], lg36[:, 4 + g * 8:4 + (g + 1) * 8], gmask[:, g:g + 1], None,
                           ALU.add, None, [t_lg36, t_gmask], [t_elm])
                    S.op("dve", lambda e: e.max(out=m8[:], in_=elm[:]), [t_elm], [t_m8])
                    TS(S, "dve", mk1[:], elm[:], m8[:, 0:1], None, ALU.is_equal, None, [t_elm, t_m8], [t_mk1])
                    TS(S, "dve", mk2[:], elm[:], m8[:, 1:2], None, ALU.is_equal, None, [t_elm, t_m8], [t_mk2])
                    TT(S, "dve", rt[:, 4:5], m8[:, 1:2], m8[:, 0:1], ALU.subtract, [t_m8, t_rt], [t_rt])
                    ACT(S, rt[:, 5:6], rt[:, 4:5], AF.Exp, [t_rt], [t_rt])
                    TS(S, "dve", rt[:, 6:7], rt[:, 5:6], 1.0, None, ALU.add, None, [t_rt], [t_rt])
                    S.op("dve", lambda e: e.reciprocal(out=rt[:, 6:7], in_=rt[:, 6:7]), [t_rt], [t_rt])
                    TT(S, "dve", rt[:, 7:8], rt[:, 5:6], rt[:, 6:7], ALU.mult, [t_rt], [t_rt])
                    TT(S, "dve", mkb[:], mk1[:], mk2[:], ALU.add, [t_mk1, t_mk2], [t_mkb])
                    MM(S, P45[:, 576:608], utri[:], mkb[:], True, True, [t_utri, t_mkb], [tP45[1]])
                    MM(S, P45[:, 640:672], ones_b[:], mkb[:], True, True, [t_ones, t_mkb], [tP45[1]])
                    TT(S, "dve", posf[:], P45[:, 576:608], tot[:], ALU.add, [tP45[1], t_tot], [t_posf])
                    TT(S, "dve", tot[:], P45[:, 640:672], tot[:], ALU.add, [tP45[1], t_tot], [t_tot])
                    TS(S, "dve", vld[:], posf[:], float(CAP), None, ALU.is_lt, None, [t_posf], [t_vld])
                    TT(S, "dve", posf[:], posf[:], sbase[:], ALU.add, [t_posf, t_sbase], [t_posf])
                    TS(S, "dve", posf[:], posf[:], -float(NSLOT), None, ALU.add, None, [t_posf], [t_posf])
                    TT(S, "dve", posf[:], posf[:], vld[:], ALU.mult, [t_posf, t_vld], [t_posf])
                    TS(S, "dve", posf[:], posf[:], float(NSLOT), None, ALU.add, None, [t_posf], [t_posf])
                    for k, mk, tmk in ((0, mk1, t_mk1), (1, mk2, t_mk2)):
                        TT(S, "dve", scr32[:], mk[:], posf[:], ALU.mult, [tmk, t_posf], [t_scr32])
                        S.op("dve", (lambda a: (lambda e: e.reduce_sum(out=a, in_=scr32[:], axis=AX.X)))(slf[:, k:k + 1]),
                             [t_scr32], [t_slf])
                        TT(S, "dve", scr32[:], mk[:], vld[:], ALU.mult, [tmk, t_vld], [t_scr32])
                        S.op("dve", (lambda a: (lambda e: e.reduce_sum(out=a, in_=scr32[:], axis=AX.X)))(rt[:, 8 + k:9 + k]),
                             [t_scr32], [t_rt])
                    CP(S, "dve", slot_i[:, tile_i, :], slf[:], [t_slf], [t_slot[tile_i]])
                    TT(S, "dve", rt[:, 10:12], rt[:, 6:8], rt[:, 8:10], ALU.mult, [t_rt], [t_rt])
                    TS(S, "dve", slot_w[:, tile_i, :], rt[:, 10:12], rt[:, 3:4], None, ALU.mult, None, [t_rt], [t_slot[tile_i]])
                    for k in range(2):
                        S.dma("pool", (lambda a, b: (lambda e: e.indirect_dma_start(
                            out=xe_d, out_offset=bass.IndirectOffsetOnAxis(ap=a, axis=0), in_=b, in_offset=None,
                            bounds_check=_bc_reg(e, 1), oob_is_err=False)))(slot_i[:, tile_i, k:k + 1], x1b[:]),
                            reads=[t_x1b, t_slot[tile_i], t_xez], writes=[], chan=t_x1b)
                S.pending = S.defer
                S.defer = None
            if DBG in ("a", "b", "c", "d", "e"):
                S.dma("sp", lambda e: e.dma_start(out=out_d[0:128, :], in_=xs[:, 0, 0, :]), reads=[t_xs[0][0]], writes=[], chan=t_xs[0][0])
            S.barrier()
            S.emit_all()
        S.stack = st
        if DBG is not None:
            return nc

        with contextlib.ExitStack() as st2:
            S.stack = st2
            NST = CAP // 128
            wg = S.sb("wg", [128, 2, 8, DE], BF16); t_wg = [S.tok(), S.tok()]
            wu = S.sb("wu", [128, 2, 8, DE], BF16); t_wu = [S.tok(), S.tok()]
            wd = S.sb("wd", [128, 2, 4, D], BF16); t_wd = [S.tok(), S.tok()]
            xet = S.sb("xet", [128, 2, NST, D], BF16); t_xet = [S.tok(), S.tok()]
            xeT = S.sb("xeT", [128, 2, 8, CAP], BF16); t_xeT = [[S.tok() for _ in range(NST)] for _ in range(2)]
            hT = S.sb("hT", [128, 2, 4, CAP], BF16); t_hT = [[[S.tok() for _ in range(2)] for _ in range(4)] for _ in range(2)]
            sgt = S.sb("sgt", [128, 2, 384], F32); t_sgt = [S.tok(), S.tok()]
            yo = S.sb("yo", [128, 2, D], BF16); t_yo = [S.tok(), S.tok()]

            def load_w(e):
                p = e % 2
                LOAD(S, "pool", wg[:, p], wg_d[e].rearrange("(kc p) c -> p kc c", p=128), t_wg[p])
                LOAD(S, "pool", wu[:, p], wu_d[e].rearrange("(kc p) c -> p kc c", p=128), t_wu[p])
                LOAD(S, "pool", wd[:, p], wd_d[e].rearrange("(kc p) c -> p kc c", p=128), t_wd[p])

            def load_xe(e):
                p = e % 2
                LOAD(S, "sp", xet[:, p], xe_d[e * CAP:(e + 1) * CAP, :].rearrange("(t p) d -> p t d", p=128), t_xet[p])

            def transpose_tile(e, s):
                p = e % 2
                for kc in range(8):
                    TR(S, P2[:, kc * 128:(kc + 1) * 128], xet[:, p, s, kc * 128:(kc + 1) * 128], ident_b[:],
                       [t_xet[p], t_idb], [tP2])
                CP(S, "dve", xeT[:, p, :, s * 128:(s + 1) * 128], P2[:].rearrange("p (k c) -> p k c", c=128),
                   [tP2], [t_xeT[p][s]])

            load_w(0)
            load_xe(0)
            for s in range(NST):
                transpose_tile(0, s)
            for e in range(NE):
                p = e % 2
                if e + 1 < NE:
                    load_w(e + 1)
                    load_xe(e + 1)
                for dc in range(4):
                    dcs = slice(dc * 128, (dc + 1) * 128)
                    for hf in range(2):
                        cs = slice(hf * 384, (hf + 1) * 384)
                        rd = [t_xeT[p][hf * 3 + i] for i in range(3)]
                        u = (dc * 2 + hf) % 2
                        pg = (PA[:, 0:384], PA[:, 512:896])[u]
                        pu = (P1, P6)[u][:, 0:384]
                        tpg = ([tPA[0]], [tPA[1]])[u]
                        tpu = ([tP1], [tP6])[u]
                        for kc in range(8):
                            MM(S, pg, wg[:, p, kc, dcs], xeT[:, p, kc, cs], kc == 0, kc == 7, [t_wg[p]] + rd, tpg)
                        for kc in range(8):
                            MM(S, pu, wu[:, p, kc, dcs], xeT[:, p, kc, cs], kc == 0, kc == 7, [t_wu[p]] + rd, tpu)
                        ACT(S, sgt[:, u, :], pg, AF.Silu, tpg, [t_sgt[u]])
                        TT(S, "dve", hT[:, p, dc, cs], pu, sgt[:, u, :], ALU.mult, list(tpu) + [t_sgt[u]], [t_hT[p][dc][hf]])
                for s in range(NST):
                    q = s % 2
                    hf = s // 3
                    for half in range(2):
                        for dc in range(4):
                            MM(S, P45[:, half * 512:(half + 1) * 512], hT[:, p, dc, s * 128:(s + 1) * 128],
                               wd[:, p, dc, half * 512:(half + 1) * 512], dc == 0, dc == 3,
                               [t_hT[p][dc][hf], t_wd[p]], [tP45[half]])
                    if e + 1 < NE:
                        transpose_tile(e + 1, s)
                    CP(S, "act", yo[:, q, 0:512], P45[:, 0:512], [tP45[0]], [t_yo[q]])
                    CP(S, "dve", yo[:, q, 512:1024], P45[:, 512:1024], [tP45[1]], [t_yo[q]])
                    r0 = e * CAP + s * 128
                    S.dma("sp", (lambda a, b: (lambda e_: e_.dma_start(out=a, in_=b)))(yb_d[r0:r0 + 128, :], yo[:, q, :]),
                          reads=[t_yo[q]], writes=[], chan=t_yo[q])
            S.barrier()
            S.emit_all()
        S.stack = st

        with contextlib.ExitStack() as st3:
            S.stack = st3
            ln2g = S.sb("ln2g", [128, D], F32); t_ln2g = S.tok()
            ln2b = S.sb("ln2b", [128, D], F32); t_ln2b = S.tok()
            LOAD(S, "sp", ln2g[:], ln2g_d, t_ln2g)
            LOAD(S, "sp", ln2b[:], ln2b_d, t_ln2b)
            NG = 4
            y0 = S.sb("y0", [128, NG, D], BF16); t_y0 = [S.tok() for _ in range(NG)]
            y1 = S.sb("y1", [128, NG, D], BF16); t_y1 = [S.tok() for _ in range(NG)]
            xa = S.sb("xa", [128, NG, D], F32); t_xa = [S.tok() for _ in range(NG)]
            h2 = S.sb("h2", [128, 2, D], F32); t_h2 = [S.tok(), S.tok()]
            hn = S.sb("hn", [128, 2, D], F32); t_hn = [S.tok(), S.tok()]
            ho = S.sb("ho", [128, 2, D], F32); t_ho = [S.tok(), S.tok()]
            st2_ = S.sb("st2", [128, 2, 6], F32); t_st2 = S.tok()
            mv2 = S.sb("mv2", [128, 2], F32); t_mv2 = S.tok()
            rs2 = S.sb("rs2", [128, 2], F32); t_rs2 = S.tok()
            for q in range(NG):
                S.op("dve", (lambda a: (lambda e: e.memset(a, 0.0)))(y0[:, q, :]), [], [t_y0[q]])
                S.op("dve", (lambda a: (lambda e: e.memset(a, 0.0)))(y1[:, q, :]), [], [t_y1[q]])

            def gather(ti):
                q = ti % NG
                for k, (yy, tyy) in enumerate(((y0, t_y0), (y1, t_y1))):
                    S.dma("pool", (lambda a, b: (lambda e: e.indirect_dma_start(
                        out=a, out_offset=None, in_=yb_d, in_offset=bass.IndirectOffsetOnAxis(ap=b, axis=0),
                        bounds_check=_bc_reg(e, 3), oob_is_err=False)))(yy[:, q, :], slot_i[:, ti, k:k + 1]),
                        reads=[t_slot[ti]], writes=[tyy[q]], chan=tyy[q])
                LOAD(S, "sp", xa[:, q, :], x1a_d[ti * 128:(ti + 1) * 128, :], t_xa[q])

            for ti in range(min(NG - 1, NTILE)):
                gather(ti)
            for ti in range(NTILE):
                g = ti % NG
                q = ti % 2
                if ti + NG - 1 < NTILE:
                    gather(ti + NG - 1)
                ACT(S, xa[:, g, :], xa[:, g, :], AF.Copy, [t_xa[g]], [t_xa[g]], scale=ALPHA)
                STT(S, hn[:, q, :], y0[:, g, :], slot_w[:, ti, 0:1], xa[:, g, :], ALU.mult, ALU.add,
                    [t_y0[g], t_slot[ti], t_xa[g]], [t_hn[q]])
                STT(S, h2[:, q, :], y1[:, g, :], slot_w[:, ti, 1:2], hn[:, q, :], ALU.mult, ALU.add,
                    [t_y1[g], t_slot[ti], t_hn[q]], [t_h2[q]])
                for half in range(2):
                    S.op("dve", (lambda a, b: (lambda e: e.bn_stats(out=a, in_=b)))(
                        st2_[:, half, :], h2[:, q, half * 512:(half + 1) * 512]), [t_h2[q]], [t_st2])
                S.op("dve", lambda e: e.bn_aggr(out=mv2[:], in_=st2_[:].rearrange("p a b -> p (a b)")), [t_st2], [t_mv2])
                ACT(S, rs2[:, 0:1], mv2[:, 1:2], AF.Sqrt, [t_mv2], [t_rs2], bias=LN_EPS)
                S.op("dve", lambda e: e.reciprocal(out=rs2[:, 0:1], in_=rs2[:, 0:1]), [t_rs2], [t_rs2])
                STT(S, rs2[:, 1:2], mv2[:, 0:1], -1.0, rs2[:, 0:1], ALU.mult, ALU.mult, [t_mv2, t_rs2], [t_rs2])
                ACT(S, hn[:, q, :], h2[:, q, :], AF.Identity, [t_h2[q], t_rs2], [t_hn[q]], bias=rs2[:, 1:2], scale=rs2[:, 0:1])
                TT(S, "dve", hn[:, q, :], hn[:, q, :], ln2g[:], ALU.mult, [t_hn[q], t_ln2g], [t_hn[q]])
                TT(S, "dve", ho[:, q, :], hn[:, q, :], ln2b[:], ALU.add, [t_hn[q], t_ln2b], [t_ho[q]])
                S.dma("sp", (lambda a, b: (lambda e: e.dma_start(out=a, in_=b)))(out_d[ti * 128:(ti + 1) * 128, :], ho[:, q, :]),
                      reads=[t_ho[q]], writes=[], chan=t_ho[q])
            S.barrier()
            S.emit_all()
        S.stack = st
    return nc


def _consts():
    ident = np.eye(128, dtype=np.float32)
    s = np.arange(128)[:, None]
    t = np.arange(128)[None, :]
    bdm = ((s <= t) & ((s // 64) == (t // 64))).astype(np.float32)
    rmask = np.ones((128, TB), np.float32)
    rmask[:, ::64] = 0.0
    slopes = np.exp2(-8.0 * (np.arange(8, dtype=np.float32) + 1.0) / 8.0).astype(np.float32)
    ab = np.zeros((128, 8, 2, 128), np.float32)
    for h in range(8):
        dist_prev = (t + 128 - s).astype(np.float32)
        ab[:, h, 0, :] = np.where(dist_prev < 128, -slopes[h] * dist_prev, -BIG)
        dist_cur = (t - s).astype(np.float32)
        ab[:, h, 1, :] = np.where(dist_cur >= 0, -slopes[h] * dist_cur, -BIG)
    utri = (s < t).astype(np.float32)
    sbase = np.broadcast_to((np.arange(NE, dtype=np.float32) * CAP)[None, :], (128, NE)).copy()
    import ml_dtypes
    return dict(ident=ident, bdmask=bdm, rmask=rmask,
                abias=np.ascontiguousarray(ab.reshape(128, -1).astype(ml_dtypes.bfloat16)), utri=utri, sbase=sbase)


def _prep_shared(inputs):
    f = lambda a: np.ascontiguousarray(np.asarray(a, dtype=np.float32))
    w_in = f(inputs["w_in"])[0]
    aq0 = 2048
    perm = np.arange(PROJ_W)
    newq = []
    for j in range(4):
        for kvh in range(2):
            hd = kvh * 4 + j
            newq.extend(range(aq0 + hd * 64, aq0 + (hd + 1) * 64))
    perm[aq0:aq0 + 512] = np.array(newq)
    w_in_p = np.ascontiguousarray(w_in[:, perm])
    w_b = f(inputs["w_branch_b"])[0]
    w_b_p = np.ascontiguousarray(w_b[np.array(newq) - aq0, :])
    lbl = f(inputs["lb_logits"])
    lbt = np.concatenate([lbl[0].reshape(4, 128).T, lbl[1].reshape(4, 128).T], axis=1)
    sinks = f(inputs["sinks"])[0]
    sinkr = np.zeros((1, 512), np.float32)
    for j in range(4):
        sinkr[0, j * 128:j * 128 + 64] = sinks[j]
        sinkr[0, j * 128 + 64:(j + 1) * 128] = sinks[4 + j]
    rep = lambda v: np.ascontiguousarray(np.broadcast_to(f(v).reshape(1, -1), (128, f(v).size)))
    wr = np.concatenate([f(inputs["router_group_w"])[0], f(inputs["router_expert_w"])[0]], axis=1)
    rbv = np.concatenate([f(inputs["router_group_b"])[0], f(inputs["router_expert_b"])[0]], axis=0)
    d = dict(
        w_in=w_in_p, lbt=np.ascontiguousarray(lbt), nw=f(inputs["hg_norm_w"])[0].reshape(128, 1).copy(),
        sinkr=sinkr, w_a=f(inputs["w_branch_a"])[0], w_b=w_b_p, w_o=f(inputs["w_out"])[0],
        ln1g=rep(inputs["ln1_g"][0]), ln1b=rep(inputs["ln1_b"][0]), ln2g=rep(inputs["ln2_g"][0]), ln2b=rep(inputs["ln2_b"][0]),
        wr=np.ascontiguousarray(wr), rb=rep(rbv),
        wg=f(inputs["w_exp_gate"])[0], wu=f(inputs["w_exp_up"])[0], wd=f(inputs["w_exp_down"])[0],
    )
    d.update(_consts())
    return d


def kernel(**inputs):
    x = np.asarray(inputs["x"], dtype=np.float32)
    B, SQ, _ = x.shape
    per = B // N_CORES
    shared = _prep_shared(inputs)
    nc = build(NSEQ=per, SEQ=SQ)
    in_maps = []
    for c in range(N_CORES):
        m = dict(shared)
        m["x"] = np.ascontiguousarray(x[c * per:(c + 1) * per].reshape(per * SQ, D))
        in_maps.append(m)
    res = run_bass_kernel_spmd(nc, in_maps, core_ids=list(range(N_CORES)))
    outs = [np.asarray(r["out"], dtype=np.float32).reshape(per, SQ, D) for r in res.results]
    return np.concatenate(outs, axis=0)
```
